# Optimizing a Trainium2 kernel written in Bass

```python
import jax, jax.numpy as jnp
from jax import lax
import numpy as np

D_MODEL = 1024
BATCH = 16
SEQ = 4096
DEPTH = 2

N_MIXERS = 2
N_META = 16
HEAD_DIM = 64
N_HEADS = 16
N_KV_HEADS = 2
GROUP = N_HEADS // N_KV_HEADS
Q_COLS = N_HEADS * HEAD_DIM
KV_COLS = N_KV_HEADS * HEAD_DIM
ROT_DIM = HEAD_DIM // 4
ROPE_THETA = 500000.0
ATTN_SCALE = HEAD_DIM ** -0.5
WINDOW = 128
BLOCK = 128
IDX_HEADS = 8
IDX_DIM = 64
IDX_ROT_DIM = IDX_DIM // 2
IDX_SCALE = IDX_DIM ** -0.5
IDX_W_SCALE = IDX_HEADS ** -0.5
TOPK_MAX = 256
IN_COLS_A = Q_COLS + 2 * KV_COLS
IN_COLS_B = Q_COLS + 2 * KV_COLS + IDX_HEADS * IDX_DIM + IDX_DIM + IDX_HEADS
N_EXPERTS = 32
TOP_K = 4
D_FF = 1024
SWIGLU_LIMIT = 7.0
SWIGLU_ALPHA = 1.702
MOE_BLOCK = 512
ALPHA = (2 * DEPTH) ** 0.25
BETA = (8 * DEPTH) ** -0.25
LN_EPS = 1e-5
NEG = -1e30
N_LAYERS_A = (DEPTH + 1) // 2
N_LAYERS_B = DEPTH // 2

kernel_name = 'hybrid_swa_sink_dsa_moe_deepnorm'


def layer_norm(x, g, b):
    xf = x.astype(jnp.float32)
    mu = jnp.mean(xf, -1, keepdims=True)
    var = jnp.mean(jnp.square(xf - mu), -1, keepdims=True)
    return ((xf - mu) * lax.rsqrt(var + LN_EPS) * g + b).astype(x.dtype)


def rope(x, pos, rot_dim):
    half = rot_dim // 2
    inv_freq = ROPE_THETA ** (-jnp.arange(half, dtype=jnp.float32) / half)
    ang = pos.astype(jnp.float32)[:, None] * inv_freq[None, :]
    cos = jnp.cos(ang)[:, None, :]
    sin = jnp.sin(ang)[:, None, :]
    x1 = x[..., :half].astype(jnp.float32)
    x2 = x[..., half:rot_dim].astype(jnp.float32)
    rot = jnp.concatenate([x1 * cos - x2 * sin, x2 * cos + x1 * sin], -1).astype(x.dtype)
    return jnp.concatenate([rot, x[..., rot_dim:]], -1)


def to_blocks(a):
    B, S = a.shape[:2]
    return jnp.moveaxis(a.reshape((B, S // BLOCK, BLOCK) + a.shape[2:]), 1, 0)


def from_blocks(a):
    a = jnp.moveaxis(a, 0, 1)
    return a.reshape((a.shape[0], a.shape[1] * a.shape[2]) + a.shape[3:])


def meta_causal_mask():
    i = jnp.arange(N_META)
    return i[None, :] <= i[:, None]


def split_heads(q, k, v, L):
    B = q.shape[0]
    pos = jnp.arange(L)
    q = rope(q.reshape(B, L, N_HEADS, HEAD_DIM), pos, ROT_DIM)
    q = q.reshape(B, L, N_KV_HEADS, GROUP, HEAD_DIM)
    k = rope(k.reshape(B, L, N_KV_HEADS, HEAD_DIM), pos, ROT_DIM)
    v = v.reshape(B, L, N_KV_HEADS, HEAD_DIM)
    return q, k, v


def swa_sink_mixer(h, w_in, b_in, sinks, w_out):
    B, L, _ = h.shape
    proj = jnp.einsum('bld,de->ble', h, w_in) + b_in
    q, k, v = jnp.split(proj, [Q_COLS, Q_COLS + KV_COLS], axis=-1)
    q, k, v = split_heads(q, k, v, L)
    sink = sinks.reshape(N_KV_HEADS, GROUP).astype(jnp.float32)[None, :, :, None, None]

    def probs_with_sink(logits):
        s = jnp.broadcast_to(sink, logits.shape[:-1] + (1,))
        p = jax.nn.softmax(jnp.concatenate([logits, s], -1), axis=-1)
        return p[..., :-1]

    qm, km, vm = q[:, :N_META], k[:, :N_META], v[:, :N_META]
    s_mm = jnp.einsum('bikgd,bjkd->bkgij', qm, km, preferred_element_type=jnp.float32) * ATTN_SCALE
    s_mm = jnp.where(meta_causal_mask(), s_mm, NEG)
    out_m = jnp.einsum('bkgij,bjkd->bikgd', probs_with_sink(s_mm).astype(v.dtype), vm)

    qb = to_blocks(q[:, N_META:])
    kb = to_blocks(k[:, N_META:])
    vb = to_blocks(v[:, N_META:])
    k_band = jnp.concatenate([jnp.concatenate([jnp.zeros_like(kb[:1]), kb[:-1]], 0), kb], 2)
    v_band = jnp.concatenate([jnp.concatenate([jnp.zeros_like(vb[:1]), vb[:-1]], 0), vb], 2)
    nb = qb.shape[0]
    ii = jnp.arange(BLOCK)[:, None]
    jj = jnp.arange(2 * BLOCK)[None, :]
    rel = BLOCK + ii - jj
    band_ok = (rel >= 0) & (rel < WINDOW)

    def block_fn(args):
        q_blk, k_blk, v_blk, n = args
        s_band = jnp.einsum('bikgd,bjkd->bkgij', q_blk, k_blk, preferred_element_type=jnp.float32) * ATTN_SCALE
        valid = band_ok & ((n > 0) | (jj >= BLOCK))
        s_band = jnp.where(valid, s_band, NEG)
        s_meta = jnp.einsum('bikgd,bmkd->bkgim', q_blk, km, preferred_element_type=jnp.float32) * ATTN_SCALE
        p = probs_with_sink(jnp.concatenate([s_meta, s_band], -1)).astype(v_blk.dtype)
        return (jnp.einsum('bkgim,bmkd->bikgd', p[..., :N_META], vm)
                + jnp.einsum('bkgij,bjkd->bikgd', p[..., N_META:], v_blk))

    out_r = lax.map(block_fn, (qb, k_band, v_band, jnp.arange(nb)))
    out = jnp.concatenate([out_m, from_blocks(out_r)], 1).reshape(B, L, Q_COLS)
    return jnp.einsum('ble,ed->bld', out, w_out)


def dsa_mixer(h, w_in, b_in, idx_k_g, idx_k_b, w_out):
    B, L, _ = h.shape
    S = L - N_META
    topk = min(TOPK_MAX, S // 4)
    proj = jnp.einsum('bld,de->ble', h, w_in) + b_in
    cuts = np.cumsum([Q_COLS, KV_COLS, KV_COLS, IDX_HEADS * IDX_DIM, IDX_DIM]).tolist()
    q, k, v, qi, ki, wi = jnp.split(proj, cuts, axis=-1)
    q, k, v = split_heads(q, k, v, L)
    pos = jnp.arange(L)
    qi = rope(qi.reshape(B, L, IDX_HEADS, IDX_DIM), pos, IDX_ROT_DIM)
    ki = rope(layer_norm(ki, idx_k_g, idx_k_b)[:, :, None, :], pos, IDX_ROT_DIM)[:, :, 0]
    wi = wi * IDX_W_SCALE

    qm, km, vm = q[:, :N_META], k[:, :N_META], v[:, :N_META]
    s_mm = jnp.einsum('bikgd,bjkd->bkgij', qm, km, preferred_element_type=jnp.float32) * ATTN_SCALE
    s_mm = jnp.where(meta_causal_mask(), s_mm, NEG)
    out_m = jnp.einsum('bkgij,bjkd->bikgd', jax.nn.softmax(s_mm, -1).astype(v.dtype), vm)

    kr, vr = k[:, N_META:], v[:, N_META:]
    kir = ki[:, N_META:]
    s_idx = jnp.arange(S)
    qb = to_blocks(q[:, N_META:])
    qib = to_blocks(qi[:, N_META:])
    wib = to_blocks(wi[:, N_META:])
    nb = qb.shape[0]
    gather_rows = jax.vmap(lambda arr, idx: arr[idx])

    def block_fn(args):
        q_blk, qi_blk, wi_blk, n = args
        t = n * BLOCK + jnp.arange(BLOCK)
        dots = jnp.einsum('bihd,bsd->bhis', qi_blk, kir, preferred_element_type=jnp.float32) * IDX_SCALE
        score = jnp.einsum('bhis,bih->bis', jax.nn.relu(dots), wi_blk.astype(jnp.float32))
        score = jnp.where(s_idx[None, :] <= t[:, None], score, NEG)
        _, sel = lax.top_k(score, topk)
        sel_ok = sel <= t[None, :, None]
        k_sel = gather_rows(kr, sel)
        v_sel = gather_rows(vr, sel)
        s_sel = jnp.einsum('bikgd,bijkd->bkgij', q_blk, k_sel, preferred_element_type=jnp.float32) * ATTN_SCALE
        s_sel = jnp.where(sel_ok[:, None, None], s_sel, NEG)
        s_meta = jnp.einsum('bikgd,bmkd->bkgim', q_blk, km, preferred_element_type=jnp.float32) * ATTN_SCALE
        p = jax.nn.softmax(jnp.concatenate([s_meta, s_sel], -1), -1).astype(v_sel.dtype)
        return (jnp.einsum('bkgim,bmkd->bikgd', p[..., :N_META], vm)
                + jnp.einsum('bkgij,bijkd->bikgd', p[..., N_META:], v_sel))

    out_r = lax.map(block_fn, (qb, qib, wib, jnp.arange(nb)))
    out = jnp.concatenate([out_m, from_blocks(out_r)], 1).reshape(B, L, Q_COLS)
    return jnp.einsum('ble,ed->bld', out, w_out)


def moe_ffn(h, w_router, b_router, w_gu, b_gu, w_down, b_down):
    B, L, D = h.shape
    x = h.reshape(-1, D)
    N = x.shape[0]
    logits = (jnp.einsum('nd,de->ne', x, w_router) + b_router).astype(jnp.float32)
    top_logit, top_e = lax.top_k(logits, TOP_K)
    gates = jax.nn.softmax(top_logit, -1)
    A = N * TOP_K
    flat_e = top_e.reshape(-1)
    flat_tok = jnp.repeat(jnp.arange(N, dtype=jnp.int32), TOP_K)
    flat_g = gates.reshape(-1)
    order = jnp.argsort(flat_e)
    e_s, tok_s, g_s = flat_e[order], flat_tok[order], flat_g[order]
    counts = jnp.bincount(flat_e, length=N_EXPERTS)
    padded = (counts + MOE_BLOCK - 1) // MOE_BLOCK * MOE_BLOCK
    start = jnp.cumsum(counts) - counts
    pend = jnp.cumsum(padded)
    pstart = pend - padded
    dest = pstart[e_s] + (jnp.arange(A) - start[e_s])
    P = (-(-A // MOE_BLOCK) + N_EXPERTS) * MOE_BLOCK
    nblk = P // MOE_BLOCK
    tok_pad = jnp.full((P,), N, jnp.int32).at[dest].set(tok_s)
    g_pad = jnp.zeros((P,), jnp.float32).at[dest].set(g_s)
    blk_e = jnp.minimum(jnp.searchsorted(pend, jnp.arange(nblk) * MOE_BLOCK, side='right'), N_EXPERTS - 1)
    x_pad = jnp.concatenate([x, jnp.zeros((1, D), x.dtype)], 0)

    def expert_block(args):
        tok, g, e = args
        xb = x_pad[tok]
        gu = xb @ w_gu[e] + b_gu[e]
        gate = jnp.minimum(gu[:, :D_FF], SWIGLU_LIMIT)
        up = jnp.clip(gu[:, D_FF:], -SWIGLU_LIMIT, SWIGLU_LIMIT)
        act = gate * jax.nn.sigmoid(SWIGLU_ALPHA * gate) * (up + 1)
        y = act @ w_down[e] + b_down[e]
        return y * g[:, None].astype(y.dtype)

    y = lax.map(expert_block, (tok_pad.reshape(nblk, MOE_BLOCK), g_pad.reshape(nblk, MOE_BLOCK), blk_e))
    out = jax.ops.segment_sum(y.reshape(P, D), tok_pad, num_segments=N + 1)[:N]
    return out.reshape(B, L, D)


def setup_inputs(seed: int = 0) -> dict:
    key = jax.random.key(seed)
    ks = jax.random.split(key, 24)
    nrm = lambda k, shape, s: jax.random.normal(k, shape, jnp.float32) * s
    D = D_MODEL
    return {
        'x': nrm(ks[0], (BATCH, SEQ, D), 1.0),
        'meta_tokens': nrm(ks[1], (N_META, D), 1.0),
        'w_in_a': nrm(ks[2], (N_LAYERS_A, D, IN_COLS_A), D ** -0.5),
        'b_in_a': nrm(ks[3], (N_LAYERS_A, IN_COLS_A), 0.02),
        'sinks_a': nrm(ks[4], (N_LAYERS_A, N_HEADS), 1.0),
        'w_out_a': nrm(ks[5], (N_LAYERS_A, Q_COLS, D), BETA * Q_COLS ** -0.5),
        'w_in_b': nrm(ks[6], (N_LAYERS_B, D, IN_COLS_B), D ** -0.5),
        'b_in_b': nrm(ks[7], (N_LAYERS_B, IN_COLS_B), 0.02),
        'idx_k_norm_g': 1.0 + nrm(ks[8], (N_LAYERS_B, IDX_DIM), 0.02),
        'idx_k_norm_b': nrm(ks[9], (N_LAYERS_B, IDX_DIM), 0.02),
        'w_out_b': nrm(ks[10], (N_LAYERS_B, Q_COLS, D), BETA * Q_COLS ** -0.5),
        'ln_mix_g': 1.0 + nrm(ks[11], (DEPTH, D), 0.02),
        'ln_mix_b': nrm(ks[12], (DEPTH, D), 0.02),
        'w_router': nrm(ks[13], (DEPTH, D, N_EXPERTS), D ** -0.5),
        'b_router': nrm(ks[14], (DEPTH, N_EXPERTS), 0.01),
        'w_gate_up': nrm(ks[15], (DEPTH, N_EXPERTS, D, 2 * D_FF), D ** -0.5),
        'b_gate_up': nrm(ks[16], (DEPTH, N_EXPERTS, 2 * D_FF), 0.02),
        'w_down': nrm(ks[17], (DEPTH, N_EXPERTS, D_FF, D), BETA * D_FF ** -0.5),
        'b_down': nrm(ks[18], (DEPTH, N_EXPERTS, D), 0.02),
        'ln_ffn_g': 1.0 + nrm(ks[19], (DEPTH, D), 0.02),
        'ln_ffn_b': nrm(ks[20], (DEPTH, D), 0.02),
    }


def reference(x, meta_tokens, w_in_a, b_in_a, sinks_a, w_out_a, w_in_b, b_in_b,
              idx_k_norm_g, idx_k_norm_b, w_out_b, ln_mix_g, ln_mix_b, w_router, b_router,
              w_gate_up, b_gate_up, w_down, b_down, ln_ffn_g, ln_ffn_b):
    B = x.shape[0]
    meta = jnp.broadcast_to(meta_tokens[None].astype(x.dtype), (B, N_META, x.shape[-1]))
    h = jnp.concatenate([meta, x], 1)
    for i in range(DEPTH):
        j = i // N_MIXERS
        if i % N_MIXERS == 0:
            mix = swa_sink_mixer(h, w_in_a[j], b_in_a[j], sinks_a[j], w_out_a[j])
        else:
            mix = dsa_mixer(h, w_in_b[j], b_in_b[j], idx_k_norm_g[j], idx_k_norm_b[j], w_out_b[j])
        h = layer_norm(ALPHA * h + mix, ln_mix_g[i], ln_mix_b[i])
        ffn = moe_ffn(h, w_router[i], b_router[i], w_gate_up[i], b_gate_up[i], w_down[i], b_down[i])
        h = layer_norm(ALPHA * h + ffn, ln_ffn_g[i], ln_ffn_b[i])
    return h[:, N_META:]
```

```python
import numpy as np
from contextlib import ExitStack
import ml_dtypes
import concourse.bass as bass
import concourse.mybir as mybir
from concourse.bass_utils import run_bass_kernel_spmd

F32 = mybir.dt.float32
BF16 = mybir.dt.bfloat16
AF = mybir.ActivationFunctionType
ALU = mybir.AluOpType
AX = mybir.AxisListType

D = 1024
N_META = 16
HD = 64
NH = 16
ROPE_THETA = 500000.0
ATTN_SCALE = HD ** -0.5
IDX_HEADS = 8
IDX_SCALE = 64 ** -0.5
IDX_W_SCALE = IDX_HEADS ** -0.5
TOPK_MAX = 256
SWIGLU_LIMIT = 7.0
SWIGLU_ALPHA = 1.702
DEPTH = 2
ALPHA = (2 * DEPTH) ** 0.25
LN_EPS = 1e-5
NEGM = -30000.0
N_BISECT = 20


class TB:
    __slots__ = ("t", "w", "r", "dsem", "dcnt")

    def __init__(self, t):
        self.t = t
        self.w = {}
        self.r = {}
        self.dsem = None
        self.dcnt = 0

    def __getitem__(self, idx):
        return self.t[idx]


class KC:
    def __init__(self, nc):
        self.nc = nc
        self.es = ExitStack()
        self.sem_max = {}
        self.sems = {}
        self.eng = {}
        for name, e in (("pe", nc.tensor), ("dve", nc.vector), ("act", nc.scalar),
                        ("pool", nc.gpsimd), ("sp", nc.sync)):
            s = self.es.enter_context(nc.semaphore("s_" + name))
            self.eng[name] = [e, s, 0, {}]
        self.nsem = 5
        self.uid = 0
        self.pools = {}
        self.pool_idx = {}

    def newsem(self, name):
        self.nsem += 1
        assert self.nsem < 140, "too many semaphores"
        return self.es.enter_context(self.nc.semaphore(name))

    def _deps(self, reads, writes):
        raw = {}
        oth = {}
        for b in reads:
            for s, v in b.w.items():
                if raw.get(s, 0) < v:
                    raw[s] = v
        for b in writes:
            for dd in (b.w, b.r):
                for s, v in dd.items():
                    if oth.get(s, 0) < v:
                        oth[s] = v
        return raw, oth

    def _wait(self, ename, deps):
        E = self.eng[ename]
        raw, oth = deps
        need = dict(raw)
        for s, v in oth.items():
            if need.get(s, 0) < v:
                need[s] = v
        for s, v in need.items():
            if ename == "pe" and s is E[1]:
                continue
            if E[3].get(s, 0) < v:
                E[0].wait_ge(s, v)
                E[3][s] = v

    def _commit(self, reads, writes, s, v):
        for b in writes:
            b.w = {s: v}
            b.r = {}
        for b in reads:
            if b.r.get(s, 0) < v:
                b.r[s] = v
        self.sem_max[s] = v

    def op(self, ename, reads, writes, fn):
        E = self.eng[ename]
        self._wait(ename, self._deps(reads, writes))
        ins = fn(E[0])
        E[2] += 1
        ins.then_inc(E[1], 1)
        self._commit(reads, writes, E[1], E[2])

    def dma(self, ename, out, in_, reads, writes, sb, **kw):
        E = self.eng[ename]
        self._wait(ename, self._deps(reads, writes))
        if sb.dsem is None:
            sb.dsem = {}
        if ename not in sb.dsem:
            pool = self.pools.setdefault(ename, [])
            idx = self.pool_idx.get(ename, 0)
            self.pool_idx[ename] = idx + 1
            if idx >= len(pool) and len(pool) < 44:
                self.uid += 1
                pool.append([self.newsem("d%d" % self.uid), 0])
            sb.dsem[ename] = pool[idx % len(pool)]
        ds = sb.dsem[ename]
        ins = E[0].dma_start(out=out, in_=in_, **kw)
        ds[1] += 16
        ins.then_inc(ds[0], 16)
        self._commit(reads, writes, ds[0], ds[1])

    def barrier(self):
        for ename, E in self.eng.items():
            for s, v in self.sem_max.items():
                if E[3].get(s, 0) < v:
                    E[0].wait_ge(s, v)
                    E[3][s] = v


class Phase:
    def __init__(self, k):
        self.k = k
        self.es = ExitStack()
        self.n = 0

    def __enter__(self):
        self.es.__enter__()
        self.k.pool_idx = {}
        return self

    def __exit__(self, *a):
        self.k.barrier()
        return self.es.__exit__(*a)

    def sb(self, shape, dt, name=None):
        self.n += 1
        self.k.uid += 1
        return TB(self.es.enter_context(self.k.nc.sbuf_tensor("%s_%d" % (name or "sb", self.k.uid), list(shape), dt)))

    def ps(self, shape, dt, name=None):
        self.k.uid += 1
        return TB(self.es.enter_context(self.k.nc.psum_tensor("%s_%d" % (name or "ps", self.k.uid), list(shape), dt)))


def apx(ap, pat, off=0):
    base = list(ap.ap)
    return bass.AP(tensor=ap.tensor, offset=ap.offset + off, ap=[list(base[0])] + [list(p) for p in pat])


def dram_bcast(ap_row, nparts):
    base = list(ap_row.ap)
    return bass.AP(tensor=ap_row.tensor, offset=ap_row.offset, ap=[[0, nparts]] + [list(p) for p in base[-1:]])


def build(S, NE, phases=("p0", "a1", "a2", "m0", "b1", "b2", "m1")):
    NB = S // 128
    NT = 2 * NB + 1
    R = NT * 128
    MT = NT - 1
    TOPK = min(TOPK_MAX, S // 4)
    nc = bass.Bass("TRN2", target_bir_lowering=False)

    def din(name, shape, dt=F32):
        return nc.dram_tensor(name, list(shape), dt, kind="ExternalInput").ap()

    def dscr(name, shape, dt):
        return nc.dram_tensor(name, list(shape), dt, kind="Internal").ap()

    hin = din("hin", [R, D])
    H = nc.dram_tensor("out", [R, D], F32, kind="ExternalOutput").ap()
    HT = dscr("HT", [D, R], BF16)
    lng = din("lng", [4, D])
    lnb = din("lnb", [4, D])
    ident_d = din("ident", [128, 128])
    W = {}
    for L, nq in (("a", 8), ("b", 8)):
        W["wq_" + L] = din("wq_" + L, [D, 1024])
        W["wqs_" + L] = din("wqs_" + L, [D, 1024])
        W["bq_" + L] = din("bq_" + L, [128, 8])
        W["bqs_" + L] = din("bqs_" + L, [128, 8])
        W["wk_" + L] = din("wk_" + L, [D, 128])
        W["wks_" + L] = din("wks_" + L, [D, 128])
        W["bk_" + L] = din("bk_" + L, [128, 1])
        W["bks_" + L] = din("bks_" + L, [128, 1])
        W["wo_" + L] = din("wo_" + L, [D, D])
    W["wvx_a"] = din("wvx_a", [D, 128])
    W["bvx_a"] = din("bvx_a", [1, 128])
    W["wvx_b"] = din("wvx_b", [D, 200])
    W["bvx_b"] = din("bvx_b", [1, 200])
    W["wqi"] = din("wqi", [D, 512])
    W["wqis"] = din("wqis", [D, 512])
    W["bqi"] = din("bqi", [128, 4])
    W["bqis"] = din("bqis", [128, 4])
    sinks = din("sinks", [1, 16])
    idxg = din("idxg", [1, 64])
    idxb = din("idxb", [1, 64])
    wr = din("wr", [2, D, NE])
    br = din("br", [2, NE])
    moe_layers = [l for l in (0, 1) if ("m%d" % l) in phases]
    wgu_l = {l: din("wgu%d" % l, [NE * D, 2048]) for l in moe_layers}
    bgu = din("bgu", [2, 128, NE * 16])
    wd_l = {l: din("wd%d" % l, [NE * D, D]) for l in moe_layers}
    bd = din("bd", [2, NE, D])
    cq = din("cq", [128, R])
    sq = din("sq", [128, R])
    ci = din("ci", [128, R])
    si = din("si", [128, R])
    ckt = din("ckt", [R, 16])
    skt = din("skt", [R, 16])
    mcur_d = din("mcur", [128, 128])
    mprev_d = din("mprev", [128, 128])
    mmeta_d = din("mmeta", [16, 16])
    negu_d = din("negu", [128, 128])
    QT = dscr("QT", [NT, 128, 1024], BF16)
    KT = dscr("KT", [128, R], BF16)
    VA = dscr("VA", [R, 130], BF16)
    QIT = dscr("QIT", [NT, 128, 512], BF16)
    KIT = dscr("KIT", [128, R], BF16)
    WI = dscr("WI", [R, 8], F32)

    WGB = {l: dscr("WGB%d" % l, [NE * D, 2048], BF16) for l in moe_layers}
    WDB = {l: dscr("WDB%d" % l, [NE * D, D], BF16) for l in moe_layers}

    k = KC(nc)

    def wcast_gen():
        owners = [TB(None) for _ in range(4)]
        i = 0
        for l in moe_layers:
            for r0 in range(0, NE * D, 128):
                o = owners[i % 4]
                i += 1
                k.dma("pool", WGB[l][r0:r0 + 128, :], wgu_l[l][r0:r0 + 128, :], [], [], o)
                k.dma("pool", WDB[l][r0:r0 + 128, :], wd_l[l][r0:r0 + 128, :], [], [], o)
                yield

    wc_state = {"gen": None}

    def wcast_pull(nblocks):
        if wc_state["gen"] is None:
            return
        for _ in range(nblocks):
            try:
                next(wc_state["gen"])
            except StopIteration:
                wc_state["gen"] = None
                return

    def phase_wcast():
        wc_state["gen"] = wcast_gen()

    class Epi:
        def __init__(self, ph, lnidx, nb=2):
            self.ph = ph
            self.nb = nb
            self.ln = lnidx is not None
            if self.ln:
                self.g = ph.sb([128, D], F32, "lng")
                self.b = ph.sb([128, D], F32, "lnb")
                k.dma("sp", self.g[:], dram_bcast(lng[lnidx:lnidx + 1, :], 128), [], [self.g], self.g)
                k.dma("sp", self.b[:], dram_bcast(lnb[lnidx:lnidx + 1, :], 128), [], [self.b], self.b)
            self.idb = ph.sb([128, 128], BF16, "idb")
            k.dma("pool", self.idb[:], ident_d[:, :], [], [self.idb], self.idb)
            self.st = [ph.sb([128, 2, 6], F32, "bnst") for _ in range(nb)]
            self.mv = [ph.sb([128, 4], F32, "mv") for _ in range(nb)]
            self.o = [ph.sb([128, D], F32, "epo") for _ in range(nb)]
            self.ob = [ph.sb([128, D], BF16, "epob") for _ in range(nb)]
            self.tt = [ph.sb([128, D], BF16, "eptt") for _ in range(nb)]
            self.pt = [ph.ps([128, D], BF16, "eppt") for _ in range(1)]
            self.i = 0

        def run(self, z, ti, eng2="pool"):
            i = self.i
            self.i += 1
            nb = self.nb
            st, mv, o, ob, tt = self.st[i % nb], self.mv[i % nb], self.o[i % nb], self.ob[i % nb], self.tt[i % nb]
            pt = self.pt[0]
            if self.ln:
                for c in range(2):
                    k.op("dve", [z], [st], lambda e, c=c: e.bn_stats(st[:, c, :], z[:, c * 512:(c + 1) * 512]))
                k.op("dve", [st], [mv], lambda e: e.bn_aggr(mv[:, 0:2], st[:]))
                k.op("dve", [mv], [mv], lambda e: e.tensor_scalar(out=mv[:, 2:3], in0=mv[:, 1:2], scalar1=LN_EPS,
                                                                  scalar2=None, op0=ALU.add))
                k.op("act", [mv], [mv], lambda e: e.activation(out=mv[:, 3:4], in_=mv[:, 2:3], func=AF.Sqrt))
                k.op("dve", [mv], [mv], lambda e: e.reciprocal(out=mv[:, 2:3], in_=mv[:, 3:4]))
                k.op("dve", [z, mv], [o], lambda e: e.tensor_scalar(out=o[:], in0=z[:], scalar1=mv[:, 0:1],
                                                                    scalar2=mv[:, 2:3], op0=ALU.subtract, op1=ALU.mult))
                k.op(eng2, [o, self.g], [o], lambda e: e.tensor_tensor(out=o[:], in0=o[:], in1=self.g[:], op=ALU.mult))
                k.op(eng2, [o, self.b], [o], lambda e: e.tensor_tensor(out=o[:], in0=o[:], in1=self.b[:], op=ALU.add))
                src = o
            else:
                src = z
            k.dma("pool", H[ti * 128:(ti + 1) * 128, :], src[:], [src], [], src)
            k.op("act", [src], [ob], lambda e: e.activation(out=ob[:], in_=src[:], func=AF.Copy))
            for c in range(8):
                k.op("pe", [ob, self.idb], [pt], lambda e, c=c: e.transpose(pt[:, c * 128:(c + 1) * 128], ob[:, c * 128:(c + 1) * 128], self.idb[:]))
            k.op("act", [pt], [tt], lambda e: e.activation(out=tt[:], in_=pt[:], func=AF.Copy))
            k.dma("pool", HT.rearrange("(c p) n -> p c n", p=128)[:, :, ti * 128:(ti + 1) * 128],
                  tt[:].rearrange("p (c n) -> p c n", c=8), [tt], [], tt)

    def phase_p0():
        with Phase(k) as ph:
            epi = Epi(ph, None)
            zs = [ph.sb([128, D], F32, "z0") for _ in range(2)]
            for ti in range(NT):
                z = zs[ti % 2]
                k.dma("sp", z[:], hin[ti * 128:(ti + 1) * 128, :], [], [z], z)
                epi.run(z, ti)
                wcast_pull((2 * NE * D // 128 + NT - 1) // NT)
            wcast_pull(1 << 30)

    def phase_moe(l):
        with Phase(k) as ph:
            import os
            epi = Epi(ph, None if os.environ.get("KDEBUG_NOLN") else 2 * l + 1, nb=int(os.environ.get("KDEBUG_NB", "1")))
            CW = 1024 if NT > 9 else 512
            TPC = CW // 128
            wr_sb = ph.sb([128, 8, NE], BF16, "wr")
            k.dma("pool", wr_sb[:], wr[l].rearrange("(c p) e -> p c e", p=128), [], [wr_sb], wr_sb)
            br_sb = ph.sb([128, NE], F32, "br")
            k.dma("sp", br_sb[:], dram_bcast(br[l:l + 1, :], 128), [], [br_sb], br_sb)
            bd_sb = ph.sb([32, D], BF16, "bd")
            k.dma("pool", bd_sb[0:NE, :], bd[l], [], [bd_sb], bd_sb)
            bgu_sb = ph.sb([128, NE * 16], F32, "bgu")
            k.dma("sp", bgu_sb[:], bgu[l], [], [bgu_sb], bgu_sb)
            ident = ph.sb([128, 128], F32, "ident")
            k.dma("sp", ident[:], ident_d[:, :], [], [ident], ident)
            hT = ph.sb([128, 8, CW], BF16, "hT")
            acc = ph.sb([128, TPC, D], F32, "acc")
            G = ph.sb([128, TPC, NE], F32, "G")
            GT = ph.sb([32, TPC, 128], BF16, "GT")
            lg = ph.sb([128, NE], F32, "lg")
            m8 = ph.sb([128, 8], F32, "m8")
            sm = ph.sb([128, 4], F32, "sm")
            wg_sb = [ph.sb([128, 8, 2048], BF16, "wg") for _ in range(2)]
            wd_sb = [ph.sb([128, 8, D], BF16, "wd") for _ in range(2)]
            actT = [ph.sb([128, 8, 512], BF16, "actT") for _ in range(2)]
            gc = [ph.sb([128, 512], F32, "gc") for _ in range(3)]
            sg = [ph.sb([128, 512], F32, "sg") for _ in range(3)]
            uc = [ph.sb([128, 512], F32, "uc") for _ in range(3)]
            psg = [ph.ps([128, 512], F32, "psg") for _ in range(2)]
            psu = [ph.ps([128, 512], F32, "psu") for _ in range(2)]
            psy = [ph.ps([128, 512], F32, "psy") for _ in range(2)]
            psr = ph.ps([128, 512], F32, "psr")
            wcnt = 0
            cnt_gu = 0
            cnt_y = 0
            nchunks = (NT * 128 + CW - 1) // CW
            for ch in range(nchunks):
                r0 = ch * CW
                cw = min(CW, NT * 128 - r0)
                tpc = cw // 128
                k.dma("sp", hT[:, :, 0:cw], HT.rearrange("(c p) n -> p c n", p=128)[:, :, r0:r0 + cw], [], [hT], hT)
                k.dma("sp", acc[:, 0:tpc, :], H[r0:r0 + cw, :].rearrange("(t p) d -> p t d", p=128), [], [acc], acc)
                for t in range(tpc):
                    for c in range(8):
                        k.op("pe", [hT, wr_sb], [psr], lambda e, c=c, t=t: e.matmul(
                            psr[:, 0:NE], hT[:, c, t * 128:(t + 1) * 128], wr_sb[:, c, :], start=(c == 0), stop=(c == 7)))
                    k.op("dve", [psr, br_sb], [lg], lambda e: e.tensor_tensor(out=lg[:], in0=psr[:, 0:NE], in1=br_sb[:], op=ALU.add))
                    k.op("dve", [lg], [m8], lambda e: e.max(out=m8[:], in_=lg[:]))
                    k.op("dve", [m8], [sm], lambda e: e.tensor_scalar(out=sm[:, 0:1], in0=m8[:, 0:1], scalar1=-1.0, scalar2=None, op0=ALU.mult))
                    k.op("act", [lg, sm], [G], lambda e, t=t: e.activation(out=G[:, t, :], in_=lg[:], func=AF.Exp, bias=sm[:, 0:1], scale=1.0))
                    k.op("dve", [lg, m8], [lg], lambda e: e.tensor_scalar(out=lg[:], in0=lg[:], scalar1=m8[:, 3:4], scalar2=None, op0=ALU.is_ge))
                    k.op("dve", [G, lg], [G], lambda e, t=t: e.tensor_tensor(out=G[:, t, :], in0=G[:, t, :], in1=lg[:], op=ALU.mult))
                    k.op("dve", [G], [sm], lambda e, t=t: e.tensor_reduce(out=sm[:, 1:2], in_=G[:, t, :], axis=AX.X, op=ALU.add))
                    k.op("dve", [sm], [sm], lambda e: e.reciprocal(out=sm[:, 2:3], in_=sm[:, 1:2]))
                    k.op("dve", [G, sm], [G], lambda e, t=t: e.tensor_scalar(out=G[:, t, :], in0=G[:, t, :], scalar1=sm[:, 2:3], scalar2=None, op0=ALU.mult))
                    k.op("pe", [G, ident], [psr], lambda e, t=t: e.matmul(psr[0:NE, 128:256], G[:, t, :], ident[:], start=True, stop=True))
                    k.op("act", [psr], [GT], lambda e, t=t: e.activation(out=GT[0:NE, t, :], in_=psr[0:NE, 128:256], func=AF.Copy))
                    for dh in range(2):
                        py = psy[cnt_y % 2]
                        cnt_y += 1
                        k.op("pe", [GT, bd_sb], [py], lambda e, t=t, dh=dh, py=py: e.matmul(
                            py[:], GT[0:NE, t, :], bd_sb[0:NE, dh * 512:(dh + 1) * 512], start=True, stop=True))
                        k.op("dve", [py, acc], [acc], lambda e, t=t, dh=dh, py=py: e.scalar_tensor_tensor(
                            out=acc[:, t, dh * 512:(dh + 1) * 512], in0=acc[:, t, dh * 512:(dh + 1) * 512], scalar=ALPHA,
                            in1=py[:], op0=ALU.mult, op1=ALU.add))
                for ex in range(NE):
                    wg_t = wg_sb[wcnt % 2]
                    wd_t = wd_sb[wcnt % 2]
                    wcnt += 1
                    e0 = ex * D
                    wgu = wgu_l[l]
                    wd = wd_l[l]
                    for hh in range(2):
                        k.dma("sp", wg_t[:, hh * 4:(hh + 1) * 4, :],
                              WGB[l][e0 + hh * 512:e0 + (hh + 1) * 512, :].rearrange("(c p) f -> p c f", p=128), [], [wg_t], wg_t)
                    k.dma("sp", wd_t[:], WDB[l][e0:e0 + D, :].rearrange("(c p) f -> p c f", p=128), [], [wd_t], wd_t)
                    for hf in range((cw + 511) // 512):
                        c0 = hf * 512
                        w = min(512, cw - c0)
                        aT = actT[(cnt_gu // 8) % 2]
                        for fc in range(8):
                            pg = psg[cnt_gu % 2]
                            pu = psu[cnt_gu % 2]
                            g_ = gc[cnt_gu % 3]
                            s_ = sg[cnt_gu % 3]
                            u_ = uc[cnt_gu % 3]
                            cnt_gu += 1
                            for c in range(8):
                                k.op("pe", [wg_t, hT], [pg], lambda e, c=c, fc=fc, pg=pg: e.matmul(
                                    pg[:, 0:w], wg_t[:, c, fc * 128:(fc + 1) * 128], hT[:, c, c0:c0 + w], start=(c == 0), stop=(c == 7)))
                            for c in range(8):
                                k.op("pe", [wg_t, hT], [pu], lambda e, c=c, fc=fc, pu=pu: e.matmul(
                                    pu[:, 0:w], wg_t[:, c, 1024 + fc * 128:1024 + (fc + 1) * 128], hT[:, c, c0:c0 + w], start=(c == 0), stop=(c == 7)))
                            bcol = ex * 16 + fc
                            k.op("act", [pu, bgu_sb], [u_], lambda e, pu=pu, u_=u_, bcol=bcol: e.activation(
                                out=u_[:, 0:w], in_=pu[:, 0:w], func=AF.Identity, bias=bgu_sb[:, bcol + 8:bcol + 9], scale=1.0))
                            k.op("dve", [pg, bgu_sb], [g_], lambda e, pg=pg, g_=g_, bcol=bcol: e.tensor_scalar(
                                out=g_[:, 0:w], in0=pg[:, 0:w], scalar1=bgu_sb[:, bcol:bcol + 1], scalar2=SWIGLU_LIMIT, op0=ALU.add, op1=ALU.min))
                            k.op("act", [g_], [s_], lambda e, g_=g_, s_=s_: e.activation(out=s_[:, 0:w], in_=g_[:, 0:w], func=AF.Sigmoid, scale=SWIGLU_ALPHA))
                            k.op("dve", [u_], [u_], lambda e, u_=u_: e.tensor_scalar(
                                out=u_[:, 0:w], in0=u_[:, 0:w], scalar1=-SWIGLU_LIMIT, scalar2=SWIGLU_LIMIT, op0=ALU.max, op1=ALU.min))
                            k.op("pool", [g_, s_], [g_], lambda e, g_=g_, s_=s_: e.tensor_tensor(out=g_[:, 0:w], in0=g_[:, 0:w], in1=s_[:, 0:w], op=ALU.mult))
                            k.op("dve", [g_, u_], [aT], lambda e, g_=g_, u_=u_, aT=aT, fc=fc: e.scalar_tensor_tensor(
                                out=aT[:, fc, 0:w], in0=u_[:, 0:w], scalar=1.0, in1=g_[:, 0:w], op0=ALU.add, op1=ALU.mult))
                        for t in range(w // 128):
                            tt_ = hf * 4 + t
                            for dh in range(2):
                                py = psy[cnt_y % 2]
                                cnt_y += 1
                                for fc in range(8):
                                    k.op("pe", [aT, wd_t], [py], lambda e, fc=fc, t=t, dh=dh, py=py, aT=aT: e.matmul(
                                        py[:], aT[:, fc, t * 128:(t + 1) * 128], wd_t[:, fc, dh * 512:(dh + 1) * 512], start=(fc == 0), stop=(fc == 7)))
                                k.op("dve", [py, G, acc], [acc], lambda e, tt_=tt_, dh=dh, py=py, ex=ex: e.scalar_tensor_tensor(
                                    out=acc[:, tt_, dh * 512:(dh + 1) * 512], in0=py[:], scalar=G[:, tt_, ex:ex + 1],
                                    in1=acc[:, tt_, dh * 512:(dh + 1) * 512], op0=ALU.mult, op1=ALU.add))
                for t in range(tpc):
                    zview = TBView(acc, acc[:, t, :])
                    epi.run(zview, r0 // 128 + t)


    def phase_proj(L):
        isb = (L == "b")
        NV = 200 if isb else 128
        with Phase(k) as ph:
            def wload(src, shape, pat, **kw):
                t = ph.sb(shape, BF16, "w")
                k.dma("pool", t[:], src.rearrange(pat, **kw), [], [t], t)
                return t

            def fload(src, shape):
                t = ph.sb(shape, F32, "c")
                k.dma("sp", t[:], src, [], [t], t)
                return t
            wq = wload(W["wq_" + L], [128, 8, 1024], "(c p) n -> p c n", p=128)
            wqs = wload(W["wqs_" + L], [128, 8, 1024], "(c p) n -> p c n", p=128)
            wk = wload(W["wk_" + L], [128, 8, 128], "(c p) n -> p c n", p=128)
            wks = wload(W["wks_" + L], [128, 8, 128], "(c p) n -> p c n", p=128)
            wvx = wload(W["wvx_" + L], [128, 8, NV], "(c p) n -> p c n", p=128)
            bq = fload(W["bq_" + L][:, :], [128, 8])
            bqs = fload(W["bqs_" + L][:, :], [128, 8])
            bk = fload(W["bk_" + L][:, :], [128, 1])
            bks = fload(W["bks_" + L][:, :], [128, 1])
            bvx = fload(dram_bcast(W["bvx_" + L][0:1, :], 128), [128, NV])
            if isb:
                wqi = wload(W["wqi"], [128, 8, 512], "(c p) n -> p c n", p=128)
                wqis = wload(W["wqis"], [128, 8, 512], "(c p) n -> p c n", p=128)
                bqi = fload(W["bqi"][:, :], [128, 4])
                bqis = fload(W["bqis"][:, :], [128, 4])
                ig = fload(dram_bcast(idxg[0:1, :], 128), [128, 64])
                ib = fload(dram_bcast(idxb[0:1, :], 128), [128, 64])
                ident = fload(ident_d[:, :], [128, 128])
            hTs = [ph.sb([128, 8, 512], BF16, "hT") for _ in range(2)]
            cts = [ph.sb([128, 512], F32, "ct") for _ in range(2)]
            sts = [ph.sb([128, 512], F32, "st") for _ in range(2)]
            qTs = [ph.sb([128, 8, 512], BF16, "qT") for _ in range(2)]
            kTs = [ph.sb([128, 512], BF16, "kT") for _ in range(2)]
            t1s = [ph.sb([128, 512], F32, "t1") for _ in range(2)]
            t2s = [ph.sb([128, 512], F32, "t2") for _ in range(2)]
            vxs = [ph.sb([128, NV], F32, "vx") for _ in range(2)]
            vas = [ph.sb([128, 130], BF16, "va") for _ in range(2)]
            for va in vas:
                k.op("dve", [], [va], lambda e, va=va: e.memset(va[:], 1.0))
            psA = [ph.ps([128, 512], F32, "psA") for _ in range(2)]
            psB = [ph.ps([128, 512], F32, "psB") for _ in range(2)]
            psV = [ph.ps([128, 512], F32, "psV") for _ in range(2)]
            if isb:
                cis = [ph.sb([128, 512], F32, "ci") for _ in range(2)]
                sis = [ph.sb([128, 512], F32, "si") for _ in range(2)]
                qiTs = [ph.sb([128, 4, 512], BF16, "qiT") for _ in range(2)]
                cks = [ph.sb([128, 16], F32, "ck") for _ in range(2)]
                sks = [ph.sb([128, 16], F32, "sk") for _ in range(2)]
                kns = [ph.sb([128, 64], F32, "kn") for _ in range(2)]
                kds = [ph.sb([128, 128], F32, "kd") for _ in range(2)]
                tmps = [ph.sb([128, 64], F32, "tmp") for _ in range(2)]
                kst = [ph.sb([128, 8], F32, "kst") for _ in range(2)]
                kiTs = [ph.sb([128, 128], BF16, "kiT") for _ in range(2)]
                wis = [ph.sb([128, 8], F32, "wis") for _ in range(2)]
                psT = ph.ps([128, 128], F32, "psT")
            chunks = [(r0, min(512, 2 * S - r0)) for r0 in range(0, 2 * S, 512)] + [(2 * S, 128)]
            cnt = 0
            vcnt = 0

            def rope_pair(wA, wB, bA, bB, j, hT, w, ct, st, outap, nout=128):
                nonlocal cnt
                pa, pb, t1, t2 = psA[cnt % 2], psB[cnt % 2], t1s[cnt % 2], t2s[cnt % 2]
                cnt += 1
                for c in range(8):
                    k.op("pe", [wA, hT], [pa], lambda e, c=c: e.matmul(pa[:, 0:w], wA[:, c, j * 128:(j + 1) * 128], hT[:, c, 0:w], start=(c == 0), stop=(c == 7)))
                for c in range(8):
                    k.op("pe", [wB, hT], [pb], lambda e, c=c: e.matmul(pb[:, 0:w], wB[:, c, j * 128:(j + 1) * 128], hT[:, c, 0:w], start=(c == 0), stop=(c == 7)))
                k.op("dve", [pa, bA, ct], [t1], lambda e: e.scalar_tensor_tensor(out=t1[:, 0:w], in0=pa[:, 0:w], scalar=bA[:, j:j + 1], in1=ct[:, 0:w], op0=ALU.add, op1=ALU.mult))
                k.op("dve", [pb, bB, st], [t2], lambda e: e.scalar_tensor_tensor(out=t2[:, 0:w], in0=pb[:, 0:w], scalar=bB[:, j:j + 1], in1=st[:, 0:w], op0=ALU.add, op1=ALU.mult))
                return t1, t2

            for ci_, (r0, w) in enumerate(chunks):
                hT, ct, st, qT, kT = hTs[ci_ % 2], cts[ci_ % 2], sts[ci_ % 2], qTs[ci_ % 2], kTs[ci_ % 2]
                k.dma("sp", hT[:, :, 0:w], HT.rearrange("(c p) n -> p c n", p=128)[:, :, r0:r0 + w], [], [hT], hT)
                k.dma("sp", ct[:, 0:w], cq[:, r0:r0 + w], [], [ct], ct)
                k.dma("sp", st[:, 0:w], sq[:, r0:r0 + w], [], [st], st)
                for j in range(8):
                    t1, t2 = rope_pair(wq, wqs, bq, bqs, j, hT, w, ct, st, None)
                    k.op("pool", [t1, t2], [qT], lambda e, j=j, t1=t1, t2=t2: e.tensor_tensor(out=qT[:, j, 0:w], in0=t1[:, 0:w], in1=t2[:, 0:w], op=ALU.add))
                for i in range(w // 128):
                    ti = r0 // 128 + i
                    k.dma("pool", QT[ti].rearrange("p (j t) -> p j t", j=8), qT[:, :, i * 128:(i + 1) * 128], [qT], [], qT)
                t1, t2 = rope_pair(wk, wks, bk, bks, 0, hT, w, ct, st, None)
                k.op("pool", [t1, t2], [kT], lambda e, t1=t1, t2=t2: e.tensor_tensor(out=kT[:, 0:w], in0=t1[:, 0:w], in1=t2[:, 0:w], op=ALU.add))
                k.dma("pool", KT[:, r0:r0 + w], kT[:, 0:w], [kT], [], kT)
                if isb:
                    cit, sit, qiT = cis[ci_ % 2], sis[ci_ % 2], qiTs[ci_ % 2]
                    k.dma("sp", cit[:, 0:w], ci[:, r0:r0 + w], [], [cit], cit)
                    k.dma("sp", sit[:, 0:w], si[:, r0:r0 + w], [], [sit], sit)
                    for j in range(4):
                        t1, t2 = rope_pair(wqi, wqis, bqi, bqis, j, hT, w, cit, sit, None)
                        k.op("pool", [t1, t2], [qiT], lambda e, j=j, t1=t1, t2=t2: e.tensor_tensor(out=qiT[:, j, 0:w], in0=t1[:, 0:w], in1=t2[:, 0:w], op=ALU.add))
                    for i in range(w // 128):
                        ti = r0 // 128 + i
                        k.dma("pool", QIT[ti].rearrange("p (j t) -> p j t", j=4), qiT[:, :, i * 128:(i + 1) * 128], [qiT], [], qiT)
                for i in range(w // 128):
                    ti = r0 // 128 + i
                    pv, vx, va = psV[vcnt % 2], vxs[vcnt % 2], vas[vcnt % 2]
                    for c in range(8):
                        k.op("pe", [hT, wvx], [pv], lambda e, c=c: e.matmul(pv[:, 0:NV], hT[:, c, i * 128:(i + 1) * 128], wvx[:, c, :], start=(c == 0), stop=(c == 7)))
                    k.op("dve", [pv, bvx], [vx], lambda e: e.tensor_tensor(out=vx[:], in0=pv[:, 0:NV], in1=bvx[:], op=ALU.add))
                    k.op("act", [vx], [va], lambda e: e.activation(out=va[:, 0:130].rearrange("p (g c) -> p g c", g=2)[:, :, 0:64],
                                                                   in_=vx[:, 0:128].rearrange("p (g c) -> p g c", g=2), func=AF.Copy))
                    k.dma("pool", VA[ti * 128:(ti + 1) * 128, :], va[:], [va], [], va)
                    if isb:
                        ck, sk, kn, kd, tmp, ks, kiT, wi_ = (cks[vcnt % 2], sks[vcnt % 2], kns[vcnt % 2], kds[vcnt % 2],
                                                               tmps[vcnt % 2], kst[vcnt % 2], kiTs[vcnt % 2], wis[vcnt % 2])
                        k.dma("sp", ck[:], ckt[ti * 128:(ti + 1) * 128, :], [], [ck], ck)
                        k.dma("sp", sk[:], skt[ti * 128:(ti + 1) * 128, :], [], [sk], sk)
                        k.op("dve", [vx], [tmp], lambda e: e.bn_stats(tmp[:, 0:6], vx[:, 128:192]))
                        k.op("dve", [tmp], [ks], lambda e: e.bn_aggr(ks[:, 0:2], tmp[:, 0:6]))
                        k.op("dve", [ks], [ks], lambda e: e.tensor_scalar(out=ks[:, 2:3], in0=ks[:, 1:2], scalar1=LN_EPS, scalar2=None, op0=ALU.add))
                        k.op("act", [ks], [ks], lambda e: e.activation(out=ks[:, 3:4], in_=ks[:, 2:3], func=AF.Sqrt))
                        k.op("dve", [ks], [ks], lambda e: e.reciprocal(out=ks[:, 2:3], in_=ks[:, 3:4]))
                        k.op("dve", [vx, ks], [kn], lambda e: e.tensor_scalar(out=kn[:], in0=vx[:, 128:192], scalar1=ks[:, 0:1], scalar2=ks[:, 2:3], op0=ALU.subtract, op1=ALU.mult))
                        k.op("pool", [kn, ig], [kn], lambda e: e.tensor_tensor(out=kn[:], in0=kn[:], in1=ig[:], op=ALU.mult))
                        k.op("pool", [kn, ib], [kn], lambda e: e.tensor_tensor(out=kn[:], in0=kn[:], in1=ib[:], op=ALU.add))
                        k.op("pool", [kn, ck], [tmp], lambda e: e.tensor_tensor(out=tmp[:, 0:16], in0=kn[:, 0:16], in1=ck[:], op=ALU.mult))
                        k.op("pool", [kn, sk], [tmp], lambda e: e.tensor_tensor(out=tmp[:, 16:32], in0=kn[:, 16:32], in1=sk[:], op=ALU.mult))
                        k.op("pool", [kn, ck], [tmp], lambda e: e.tensor_tensor(out=tmp[:, 32:48], in0=kn[:, 16:32], in1=ck[:], op=ALU.mult))
                        k.op("pool", [kn, sk], [tmp], lambda e: e.tensor_tensor(out=tmp[:, 48:64], in0=kn[:, 0:16], in1=sk[:], op=ALU.mult))
                        k.op("pool", [tmp], [kd], lambda e: e.tensor_tensor(out=kd[:, 0:16], in0=tmp[:, 0:16], in1=tmp[:, 16:32], op=ALU.subtract))
                        k.op("pool", [tmp], [kd], lambda e: e.tensor_tensor(out=kd[:, 16:32], in0=tmp[:, 32:48], in1=tmp[:, 48:64], op=ALU.add))
                        k.op("pool", [kn], [kd], lambda e: e.tensor_copy(kd[:, 32:64], kn[:, 32:64]))
                        k.op("pool", [kd], [kd], lambda e: e.tensor_copy(kd[:, 64:128], kd[:, 0:64]))
                        k.op("pe", [kd, ident], [psT], lambda e: e.transpose(psT[:], kd[:], ident[:]))
                        k.op("act", [psT], [kiT], lambda e: e.activation(out=kiT[:], in_=psT[:], func=AF.Copy))
                        k.dma("pool", KIT[:, ti * 128:(ti + 1) * 128], kiT[:], [kiT], [], kiT)
                        k.op("pool", [vx], [wi_], lambda e: e.tensor_scalar(out=wi_[:], in0=vx[:, 192:200], scalar1=IDX_W_SCALE * IDX_SCALE, scalar2=None, op0=ALU.mult))
                        k.dma("pool", WI[ti * 128:(ti + 1) * 128, :], wi_[:], [wi_], [], wi_)
                    vcnt += 1

    def phase_attn(L, lnidx):
        isb = (L == "b")
        with Phase(k) as ph:
            epi = Epi(ph, lnidx, nb=(1 if isb else 2))
            wo_sb = ph.sb([64, 16, D], BF16, "wo")
            k.dma("pool", wo_sb[:], W["wo_" + L].rearrange("(h d) n -> d h n", d=64), [], [wo_sb], wo_sb)
            mcur = ph.sb([128, 128], BF16, "mcur")
            k.dma("pool", mcur[:], mcur_d[:, :], [], [mcur], mcur)
            mprev = ph.sb([128, 128], BF16, "mprev")
            k.dma("pool", mprev[:], mprev_d[:, :], [], [mprev], mprev)
            mmeta = ph.sb([16, 16], BF16, "mmeta")
            k.dma("pool", mmeta[:], mmeta_d[:, :], [], [mmeta], mmeta)
            ones = ph.sb([128, 64], F32, "ones")
            k.op("dve", [], [ones], lambda e: e.memset(ones[:], 1.0))
            esrow = None
            if not isb:
                sraw = ph.sb([128, 16], F32, "sraw")
                k.dma("sp", sraw[64:65, :], sinks[0:1, :], [], [sraw], sraw)
                k.op("act", [sraw], [sraw], lambda e: e.activation(out=sraw[64:65, :], in_=sraw[64:65, :], func=AF.Exp))
                esrow = ph.sb([128, 16, 128], F32, "esrow")
                k.op("dve", [sraw], [esrow], lambda e: e.tensor_copy(esrow[64:65, :, :], apx(sraw[64:65, :], [[1, 16], [0, 128]])))
            else:
                negu = ph.sb([128, 128], F32, "negu")
                k.dma("sp", negu[:], negu_d[:, :], [], [negu], negu)
                idb = ph.sb([128, 128], BF16, "idb2")
                k.dma("pool", idb[:], ident_d[:, :], [], [idb], idb)
            attnT_m = ph.sb([64, 16, 48], BF16, "attnTm")
            k.op("dve", [], [attnT_m], lambda e: e.memset(attnT_m[:], 0.0))
            qTm = ph.sb([128, 8, 128], BF16, "qTm")
            k.dma("sp", qTm[:], QT[MT].rearrange("p (j t) -> p j t", j=8), [], [qTm], qTm)
            qTs = [ph.sb([128, 1024], BF16, "qT") for _ in range(2)]
            hs = [ph.sb([128, D], F32, "h") for _ in range(2)]
            zs = [ph.sb([128, D], F32, "z") for _ in range(2)]
            attnTs = [ph.sb([64, 16, 128], BF16, "attnT") for _ in range(2)]
            PTs = [ph.sb([128, 512], BF16, "PT") for _ in range(3)]
            rrs = [ph.sb([128, 512], F32, "rr") for _ in range(2)]
            oSs = [ph.sb([64, 512], F32, "oS") for _ in range(2)]
            bcSs = [ph.sb([64, 512], F32, "bcS") for _ in range(2)]
            kTm = ph.sb([128, 16], BF16, "kTm")
            Vm = ph.sb([16, 130], BF16, "Vm")
            psS = [ph.ps([128, 512], F32, "psS") for _ in range(2)]
            psO = [ph.ps([128, 512], F32, "psO") for _ in range(1 if isb else 2)]
            psBc = ph.ps([128, 512], F32, "psBc")
            psM = ph.ps([128, 512], F32, "psM")
            if isb:
                kT_all = ph.sb([128, S], BF16, "kTall")
                kiT_all = ph.sb([128, S], BF16, "kiTall")
                V_all = ph.sb([128, NB, 130], BF16, "Vall")
                scores = [ph.sb([128, S], F32, "score") for _ in range(2)]
                maskbs = [ph.sb([128, S], BF16, "maskb") for _ in range(2)]
                maskTs = [ph.sb([128, S], BF16, "maskT") for _ in range(2)]
                rls = [ph.sb([128, 512], F32, "rl") for _ in range(2)]
                qiTs = [ph.sb([128, 512], BF16, "qiT") for _ in range(2)]
                wits = [ph.sb([128, 8], F32, "wit") for _ in range(2)]
                nbias = ph.sb([128, 1], F32, "nbias")
                k.op("dve", [], [nbias], lambda e: e.memset(nbias[:], NEGM))
                score_gs = [[TB(None) for _ in range((S + 511) // 512)] for _ in range(2)]
                bstate = [[ph.sb([128, 1], F32, "bs") for _ in range(8)] + [ph.sb([128, N_BISECT + 1], F32, "Dk")] for _ in range(2)]
                pow2 = ph.sb([128, N_BISECT + 1], F32, "pow2")
                for kk in range(N_BISECT + 1):
                    k.op("dve", [], [pow2], lambda e, kk=kk: e.memset(pow2[:, kk:kk + 1], 2.0 ** -(kk + 1)))
                psI = ph.ps([128, 512], F32, "psI")
                psIs = [psI, psM]
                psTm = ph.ps([128, 512], BF16, "psTm")
            else:
                kT2s = [ph.sb([128, 256], BF16, "kT2") for _ in range(2)]
                V2s = [ph.sb([128, 2, 130], BF16, "V2") for _ in range(2)]
            cS = 0
            cP = 0
            cO = 0
            cM = 0

            def attend(qrhs, ncol, keytiles, g, sink_ap, out_ap):
                nonlocal cS, cP, cO, cM
                po = psO[cO % len(psO)]
                rr = rrs[cO % 2]
                oS = oSs[cO % 2]
                cO += 1
                LA = 2
                nkt = len(keytiles)
                pts = [None] * nkt
                for idx in range(nkt + LA):
                    if idx < nkt:
                        kap, vap, nk, mask, rds, mrds = keytiles[idx][:6]
                        mbias = keytiles[idx][6] if len(keytiles[idx]) > 6 else None
                        ps = psS[cS % 2]
                        cS += 1
                        PT = PTs[cP % 3]
                        cP += 1
                        pts[idx] = PT
                        k.op("pe", rds + qrhs[1], [ps], lambda e: e.matmul(ps[0:nk, 0:ncol], kap, qrhs[0], start=True, stop=(mbias is None)))
                        if mbias is not None:
                            k.op("pe", mrds + [idb], [ps], lambda e: e.matmul(ps[0:nk, 0:ncol], idb[0:nk, 0:nk], mbias, start=False, stop=True))
                        k.op("act", [ps], [PT], lambda e: e.activation(out=PT[0:nk, 0:ncol], in_=ps[0:nk, 0:ncol], func=AF.Exp, scale=ATTN_SCALE))
                        if mask is not None:
                            eng = "pool" if (isb or cM % 2 == 0) else "dve"
                            cM += 1
                            k.op(eng, [PT] + mrds, [PT], lambda e: e.tensor_tensor(out=PT[0:nk, 0:ncol], in0=PT[0:nk, 0:ncol], in1=mask, op=ALU.mult))
                    j = idx - LA
                    if j >= 0:
                        kap, vap, nk, mask, rds, mrds = keytiles[j][:6]
                        PT = pts[j]
                        k.op("pe", rds + [PT], [po], lambda e: e.matmul(po[0:65, 0:ncol], vap, PT[0:nk, 0:ncol], start=(j == 0), stop=(j == nkt - 1)))
                if sink_ap is not None:
                    k.op("dve", [po, esrow], [rr], lambda e: e.tensor_tensor(out=rr[64:65, 0:ncol], in0=po[64:65, 0:ncol], in1=sink_ap, op=ALU.add))
                    k.op("dve", [rr], [rr], lambda e: e.reciprocal(out=rr[64:65, 0:ncol], in_=rr[64:65, 0:ncol]))
                elif isb:
                    bcS = bcSs[cO % 2]
                    k.op("act", [po], [rr], lambda e: e.activation(out=rr[64:65, 0:ncol], in_=po[64:65, 0:ncol], func=AF.Copy))
                    k.op("act", [rr], [rr], lambda e: e.activation(out=rr[64:65, 0:ncol], in_=rr[64:65, 0:ncol], func=AF.Ln))
                    k.op("act", [rr], [rr], lambda e: e.activation(out=rr[64:65, 0:ncol], in_=rr[64:65, 0:ncol], func=AF.Exp, scale=-1.0))
                    k.op("pe", [ones, rr], [psBc], lambda e: e.matmul(psBc[0:64, 0:ncol], ones[64:65, 0:64], rr[64:65, 0:ncol], start=True, stop=True))
                    k.op("act", [psBc], [bcS], lambda e: e.activation(out=bcS[0:64, 0:ncol], in_=psBc[0:64, 0:ncol], func=AF.Copy))
                    k.op("act", [po], [oS], lambda e: e.activation(out=oS[0:64, 0:ncol], in_=po[0:64, 0:ncol], func=AF.Copy))
                    k.op("pool", [oS, bcS], out_ap[1], lambda e: e.tensor_tensor(out=out_ap[0], in0=oS[0:64, 0:ncol], in1=bcS[0:64, 0:ncol], op=ALU.mult))
                    return
                else:
                    k.op("dve", [po], [rr], lambda e: e.reciprocal(out=rr[64:65, 0:ncol], in_=po[64:65, 0:ncol]))
                k.op("pe", [ones, rr], [psBc], lambda e: e.matmul(psBc[0:64, 0:ncol], ones[64:65, 0:64], rr[64:65, 0:ncol], start=True, stop=True))
                k.op("act", [po], [oS], lambda e: e.activation(out=oS[0:64, 0:ncol], in_=po[0:64, 0:ncol], func=AF.Copy))
                k.op("dve", [oS, psBc], out_ap[1], lambda e: e.tensor_tensor(out=out_ap[0], in0=oS[0:64, 0:ncol], in1=psBc[0:64, 0:ncol], op=ALU.mult))

            def meta_queries(b):
                for g in range(2):
                    gs = slice(g * 64, (g + 1) * 64)
                    kts = [(kTm[gs, 0:16], Vm[0:16, g * 65:(g + 1) * 65], 16, apx(mmeta[:, :], [[0, 8], [1, 16]]), [kTm, Vm], [mmeta])]
                    sink_ap = None if isb else esrow[64:65, g * 8:(g + 1) * 8, 0:16]
                    attend((qTm[gs, :, 32 * b:32 * b + 16], [qTm]), 128, kts, g, sink_ap,
                           (attnT_m[0:64, g * 8:(g + 1) * 8, 32 * b:32 * b + 16], [attnT_m]))

            def out_proj_epi(attnT, h, z, ti):
                for dh in range(2):
                    for hd in range(16):
                        k.op("pe", [attnT, wo_sb], [psM], lambda e, hd=hd: e.matmul(psM[:, :], attnT[0:64, hd, :], wo_sb[0:64, hd, dh * 512:(dh + 1) * 512], start=(hd == 0), stop=(hd == 15)))
                    k.op("dve", [h, psM], [z], lambda e: e.scalar_tensor_tensor(out=z[:, dh * 512:(dh + 1) * 512], in0=h[:, dh * 512:(dh + 1) * 512], scalar=ALPHA,
                                                                                in1=psM[:, :], op0=ALU.mult, op1=ALU.add))
                epi.run(z, ti)

            if isb:
                blocks = [(b, n) for b in range(2) for n in range(NB)]
                ri_box = [0]

                def A1(i):
                    b, n = blocks[i]
                    p = i % 2
                    ti = b * NB + n
                    Wk = (n + 1) * 128
                    score, score_g = scores[p], score_gs[p]
                    qiT, wit = qiTs[p], wits[p]
                    bst = bstate[p]
                    if n == 0:
                        k.dma("sp", kiT_all[:], KIT[:, b * S:(b + 1) * S], [], [kiT_all], kiT_all)
                    k.dma("sp", qiT[:], QIT[ti], [], [qiT], qiT)
                    k.dma("sp", wit[:], WI[ti * 128:(ti + 1) * 128, :], [], [wit], wit)
                    ngr = (Wk + 511) // 512
                    sgs = score_g[0:ngr]
                    for hh in range(8):
                        j, half = hh % 4, hh // 4
                        for gi_ in range(ngr):
                            kg0 = gi_ * 512
                            w = min(512, Wk - kg0)
                            sg_ = score_g[gi_]
                            rl = rls[ri_box[0] % 2]
                            pI = psIs[ri_box[0] % 2]
                            ri_box[0] += 1
                            k.op("pe", [qiT, kiT_all], [pI], lambda e: e.matmul(pI[:, 0:w], qiT[half * 64:(half + 1) * 64, j * 128:(j + 1) * 128],
                                                                                kiT_all[half * 64:(half + 1) * 64, kg0:kg0 + w], start=True, stop=True))
                            k.op("act", [pI], [rl], lambda e: e.activation(out=rl[:, 0:w], in_=pI[:, 0:w], func=AF.Relu))
                            if hh == 0:
                                k.op("dve", [rl, wit], [sg_], lambda e: e.tensor_scalar(out=score[:, kg0:kg0 + w], in0=rl[:, 0:w], scalar1=wit[:, 0:1], scalar2=None, op0=ALU.mult))
                            else:
                                k.op("dve", [rl, wit, sg_], [sg_], lambda e: e.scalar_tensor_tensor(out=score[:, kg0:kg0 + w], in0=rl[:, 0:w], scalar=wit[:, hh:hh + 1],
                                                                                                    in1=score[:, kg0:kg0 + w], op0=ALU.mult, op1=ALU.add))
                    b_mx, b_mn, b_d, b_a0, b_a1, b_mid, b_cnt, b_tmp, Dk = bst
                    k.op("dve", sgs, [b_mx], lambda e: e.tensor_reduce(out=b_mx[:, 0:1], in_=score[:, 0:Wk], axis=AX.X, op=ALU.max))
                    k.op("dve", sgs, [b_mn], lambda e: e.tensor_reduce(out=b_mn[:, 0:1], in_=score[:, 0:Wk], axis=AX.X, op=ALU.min))
                    dg = score_g[n // 4]
                    k.op("dve", [dg, negu], [dg], lambda e: e.tensor_tensor(out=score[:, n * 128:(n + 1) * 128], in0=score[:, n * 128:(n + 1) * 128], in1=negu[:], op=ALU.add))
                    k.op("dve", [b_mx, b_mn], [b_d], lambda e: e.tensor_tensor(out=b_d[:, 0:1], in0=b_mx[:, 0:1], in1=b_mn[:, 0:1], op=ALU.subtract))
                    k.op("dve", [b_d], [b_d], lambda e: e.tensor_scalar(out=b_d[:, 0:1], in0=b_d[:, 0:1], scalar1=2.0, scalar2=None, op0=ALU.add))
                    k.op("dve", [b_mn], [b_a0], lambda e: e.tensor_scalar(out=b_a0[:, 0:1], in0=b_mn[:, 0:1], scalar1=-1.0, scalar2=None, op0=ALU.add))
                    k.op("dve", [pow2, b_d], [Dk], lambda e: e.tensor_scalar(out=Dk[:], in0=pow2[:], scalar1=b_d[:, 0:1], scalar2=None, op0=ALU.mult))
                    k.op("dve", [b_a0, Dk], [b_mid], lambda e: e.tensor_tensor(out=b_mid[:, 0:1], in0=b_a0[:, 0:1], in1=Dk[:, 0:1], op=ALU.add))

                def A2(i):
                    b, n = blocks[i]
                    p = i % 2
                    Wk = (n + 1) * 128
                    score, score_g, junk = scores[p], score_gs[p], maskbs[p]
                    sgs = score_g[0:(Wk + 511) // 512]
                    b_mx, b_mn, b_d, b_a0, b_a1, b_mid, b_cnt, b_tmp, Dk = bstate[p]
                    b_as = [b_a0, b_a1]
                    for it in range(N_BISECT):
                        k.op("dve", sgs + [b_mid], [junk, b_cnt], lambda e: e.tensor_scalar(out=junk[:, 0:Wk], in0=score[:, 0:Wk], scalar1=b_mid[:, 0:1], scalar2=0.0,
                                                                                         op0=ALU.is_ge, op1=ALU.add, accum_out=b_cnt[:, 0:1]))
                        k.op("dve", [b_cnt, Dk], [b_tmp], lambda e: e.scalar_tensor_tensor(out=b_tmp[:, 0:1], in0=b_cnt[:, 0:1], scalar=float(TOPK) - 0.5,
                                                                                          in1=Dk[:, it:it + 1], op0=ALU.is_ge, op1=ALU.mult))
                        b_a, b_an = b_as[it % 2], b_as[(it + 1) % 2]
                        if it < N_BISECT - 1:
                            k.op("dve", [b_tmp, b_a, Dk], [b_mid], lambda e: e.tensor_scalar(out=b_mid[:, 0:1], in0=b_tmp[:, 0:1], scalar1=b_a[:, 0:1],
                                                                                           scalar2=Dk[:, it + 1:it + 2], op0=ALU.add, op1=ALU.add))
                        k.op("dve", [b_tmp, b_a], [b_an], lambda e: e.tensor_tensor(out=b_an[:, 0:1], in0=b_a[:, 0:1], in1=b_tmp[:, 0:1], op=ALU.add))

                def A3(i):
                    b, n = blocks[i]
                    p = i % 2
                    Wk = (n + 1) * 128
                    score, score_g, maskb, maskT = scores[p], score_gs[p], maskbs[p], maskTs[p]
                    sgs = score_g[0:(Wk + 511) // 512]
                    b_a = bstate[p][3 + (N_BISECT % 2)]
                    k.op("pool", sgs + [b_a], [maskb], lambda e: e.tensor_scalar(out=maskb[:, 0:Wk], in0=score[:, 0:Wk], scalar1=b_a[:, 0:1], scalar2=None, op0=ALU.is_ge))
                    for m0 in range(0, n + 1, 4):
                        mc = min(4, n + 1 - m0)
                        for mm in range(mc):
                            k.op("pe", [maskb, idb], [psTm], lambda e, mm=mm: e.transpose(psTm[:, mm * 128:(mm + 1) * 128], maskb[:, (m0 + mm) * 128:(m0 + mm + 1) * 128], idb[:]))
                        k.op("act", [psTm, nbias], [maskT], lambda e: e.activation(out=maskT[:, m0 * 128:(m0 + mc) * 128], in_=psTm[:, 0:mc * 128], func=AF.Identity,
                                                                                       bias=nbias[:, 0:1], scale=-NEGM))

                def Bst(i):
                    b, n = blocks[i]
                    p = i % 2
                    ti = b * NB + n
                    maskT = maskTs[p]
                    qT, h, z, attnT = qTs[p], hs[p], zs[p], attnTs[p]
                    if n == 0:
                        mrow = 2 * S + 32 * b
                        k.dma("sp", kTm[:], KT[:, mrow:mrow + 16], [], [kTm], kTm)
                        k.dma("sp", Vm[:], VA[mrow:mrow + 16, :], [], [Vm], Vm)
                        k.dma("sp", kT_all[:], KT[:, b * S:(b + 1) * S], [], [kT_all], kT_all)
                        k.dma("sp", V_all[:], VA[b * S:(b + 1) * S, :].rearrange("(n p) c -> p n c", p=128), [], [V_all], V_all)
                    k.dma("sp", qT[:], QT[ti], [], [qT], qT)
                    k.dma("sp", h[:], H[ti * 128:(ti + 1) * 128, :], [], [h], h)
                    for g in range(2):
                        gs = slice(g * 64, (g + 1) * 64)
                        for half in range(2):
                            kts = [(kTm[gs, 0:16], Vm[0:16, g * 65:(g + 1) * 65], 16, None, [kTm, Vm], [])]
                            for m in range(n + 1):
                                kts.append((kT_all[gs, m * 128:(m + 1) * 128], V_all[:, m, g * 65:(g + 1) * 65], 128,
                                            None, [kT_all, V_all], [maskT], apx(maskT[:, m * 128:(m + 1) * 128], [[0, 4], [1, 128]])))
                            hd0 = g * 8 + half * 4
                            attend((qT[gs, half * 512:(half + 1) * 512], [qT]), 512, kts, g, None,
                                   (attnT[0:64, hd0:hd0 + 4, :].rearrange("p h t -> p (h t)"), [attnT]))
                    out_proj_epi(attnT, h, z, ti)
                    if n == NB - 1:
                        meta_queries(b)

                A1(0)
                A2(0)
                A3(0)
                for i in range(len(blocks)):
                    if i + 1 < len(blocks):
                        A1(i + 1)
                        A2(i + 1)
                    Bst(i)
                    if i + 1 < len(blocks):
                        A3(i + 1)
                blk = len(blocks)
            else:
                blk = 0
                for b in range(2):
                    mrow = 2 * S + 32 * b
                    k.dma("sp", kTm[:], KT[:, mrow:mrow + 16], [], [kTm], kTm)
                    k.dma("sp", Vm[:], VA[mrow:mrow + 16, :], [], [Vm], Vm)
                    if isb:
                        k.dma("sp", kT_all[:], KT[:, b * S:(b + 1) * S], [], [kT_all], kT_all)
                        k.dma("sp", kiT_all[:], KIT[:, b * S:(b + 1) * S], [], [kiT_all], kiT_all)
                        k.dma("sp", V_all[:], VA[b * S:(b + 1) * S, :].rearrange("(n p) c -> p n c", p=128), [], [V_all], V_all)
                    for n in range(NB):
                        ti = b * NB + n
                        qT, h, z, attnT = qTs[blk % 2], hs[blk % 2], zs[blk % 2], attnTs[blk % 2]
                        k.dma("sp", qT[:], QT[ti], [], [qT], qT)
                        k.dma("sp", h[:], H[ti * 128:(ti + 1) * 128, :], [], [h], h)
                        if not isb:
                            kT2, V2 = kT2s[blk % 2], V2s[blk % 2]
                            if n > 0:
                                k.dma("sp", kT2[:], KT[:, (ti - 1) * 128:(ti + 1) * 128], [], [kT2], kT2)
                                k.dma("sp", V2[:], VA[(ti - 1) * 128:(ti + 1) * 128, :].rearrange("(n p) c -> p n c", p=128), [], [V2], V2)
                            else:
                                k.dma("sp", kT2[:, 128:256], KT[:, ti * 128:(ti + 1) * 128], [], [kT2], kT2)
                                k.dma("sp", V2[:, 1, :], VA[ti * 128:(ti + 1) * 128, :], [], [V2], V2)
                        else:
                            Wk = (n + 1) * 128
                            qiT, wit = qiTs[blk % 2], wits[blk % 2]
                            k.dma("sp", qiT[:], QIT[ti], [], [qiT], qiT)
                            k.dma("sp", wit[:], WI[ti * 128:(ti + 1) * 128, :], [], [wit], wit)
                            ri = 0
                            ngr = (Wk + 511) // 512
                            sgs = score_g[0:ngr]
                            for hh in range(8):
                                j, half = hh % 4, hh // 4
                                for gi_ in range(ngr):
                                    kg0 = gi_ * 512
                                    w = min(512, Wk - kg0)
                                    sg_ = score_g[gi_]
                                    rl = rls[ri % 2]
                                    pI = psIs[ri % 2]
                                    ri += 1
                                    k.op("pe", [qiT, kiT_all], [pI], lambda e: e.matmul(pI[:, 0:w], qiT[half * 64:(half + 1) * 64, j * 128:(j + 1) * 128],
                                                                                        kiT_all[half * 64:(half + 1) * 64, kg0:kg0 + w], start=True, stop=True))
                                    k.op("act", [pI], [rl], lambda e: e.activation(out=rl[:, 0:w], in_=pI[:, 0:w], func=AF.Relu))
                                    if hh == 0:
                                        k.op("dve", [rl, wit], [sg_], lambda e: e.tensor_scalar(out=score[:, kg0:kg0 + w], in0=rl[:, 0:w], scalar1=wit[:, 0:1], scalar2=None, op0=ALU.mult))
                                    else:
                                        k.op("dve", [rl, wit, sg_], [sg_], lambda e: e.scalar_tensor_tensor(out=score[:, kg0:kg0 + w], in0=rl[:, 0:w], scalar=wit[:, hh:hh + 1],
                                                                                                            in1=score[:, kg0:kg0 + w], op0=ALU.mult, op1=ALU.add))
                            k.op("dve", sgs, [b_mx], lambda e: e.tensor_reduce(out=b_mx[:, 0:1], in_=score[:, 0:Wk], axis=AX.X, op=ALU.max))
                            k.op("dve", sgs, [b_mn], lambda e: e.tensor_reduce(out=b_mn[:, 0:1], in_=score[:, 0:Wk], axis=AX.X, op=ALU.min))
                            dg = score_g[n // 4]
                            k.op("dve", [dg, negu], [dg], lambda e: e.tensor_tensor(out=score[:, n * 128:(n + 1) * 128], in0=score[:, n * 128:(n + 1) * 128], in1=negu[:], op=ALU.add))
                            k.op("dve", [b_mx, b_mn], [b_d], lambda e: e.tensor_tensor(out=b_d[:, 0:1], in0=b_mx[:, 0:1], in1=b_mn[:, 0:1], op=ALU.subtract))
                            k.op("dve", [b_d], [b_d], lambda e: e.tensor_scalar(out=b_d[:, 0:1], in0=b_d[:, 0:1], scalar1=2.0, scalar2=None, op0=ALU.add))
                            b_a = b_as[0]
                            k.op("dve", [b_mn], [b_a], lambda e: e.tensor_scalar(out=b_a[:, 0:1], in0=b_mn[:, 0:1], scalar1=-1.0, scalar2=None, op0=ALU.add))
                            k.op("dve", [pow2, b_d], [Dk], lambda e: e.tensor_scalar(out=Dk[:], in0=pow2[:], scalar1=b_d[:, 0:1], scalar2=None, op0=ALU.mult))
                            k.op("dve", [b_a, Dk], [b_mid], lambda e: e.tensor_tensor(out=b_mid[:, 0:1], in0=b_a[:, 0:1], in1=Dk[:, 0:1], op=ALU.add))
                            for it in range(N_BISECT):
                                k.op("dve", sgs + [b_mid], [junk, b_cnt], lambda e: e.tensor_scalar(out=junk[:, 0:Wk], in0=score[:, 0:Wk], scalar1=b_mid[:, 0:1], scalar2=0.0,
                                                                                                 op0=ALU.is_ge, op1=ALU.add, accum_out=b_cnt[:, 0:1]))
                                k.op("dve", [b_cnt, Dk], [b_tmp], lambda e: e.scalar_tensor_tensor(out=b_tmp[:, 0:1], in0=b_cnt[:, 0:1], scalar=float(TOPK) - 0.5,
                                                                                                  in1=Dk[:, it:it + 1], op0=ALU.is_ge, op1=ALU.mult))
                                b_a, b_an = b_as[it % 2], b_as[(it + 1) % 2]
                                if it < N_BISECT - 1:
                                    k.op("dve", [b_tmp, b_a, Dk], [b_mid], lambda e: e.tensor_scalar(out=b_mid[:, 0:1], in0=b_tmp[:, 0:1], scalar1=b_a[:, 0:1],
                                                                                                   scalar2=Dk[:, it + 1:it + 2], op0=ALU.add, op1=ALU.add))
                                k.op("dve", [b_tmp, b_a], [b_an], lambda e: e.tensor_tensor(out=b_an[:, 0:1], in0=b_a[:, 0:1], in1=b_tmp[:, 0:1], op=ALU.add))
                            b_a = b_as[N_BISECT % 2]
                            k.op("pool", sgs + [b_a], [maskb], lambda e: e.tensor_scalar(out=maskb[:, 0:Wk], in0=score[:, 0:Wk], scalar1=b_a[:, 0:1], scalar2=None, op0=ALU.is_ge))
                            for m0 in range(0, n + 1, 4):
                                mc = min(4, n + 1 - m0)
                                for mm in range(mc):
                                    k.op("pe", [maskb, idb], [psTm], lambda e, mm=mm: e.transpose(psTm[:, mm * 128:(mm + 1) * 128], maskb[:, (m0 + mm) * 128:(m0 + mm + 1) * 128], idb[:]))
                                k.op("act", [psTm], [maskT], lambda e: e.activation(out=maskT[:, m0 * 128:(m0 + mc) * 128], in_=psTm[:, 0:mc * 128], func=AF.Copy))
                        for g in range(2):
                            gs = slice(g * 64, (g + 1) * 64)
                            for half in range(2):
                                kts = [(kTm[gs, 0:16], Vm[0:16, g * 65:(g + 1) * 65], 16, None, [kTm, Vm], [])]
                                if not isb:
                                    if n > 0:
                                        kts.append((kT2[gs, 0:128], V2[:, 0, g * 65:(g + 1) * 65], 128, apx(mprev[:, :], [[0, 4], [1, 128]]), [kT2, V2], [mprev]))
                                    kts.append((kT2[gs, 128:256], V2[:, 1, g * 65:(g + 1) * 65], 128, apx(mcur[:, :], [[0, 4], [1, 128]]), [kT2, V2], [mcur]))
                                else:
                                    for m in range(n + 1):
                                        kts.append((kT_all[gs, m * 128:(m + 1) * 128], V_all[:, m, g * 65:(g + 1) * 65], 128,
                                                    apx(maskT[:, m * 128:(m + 1) * 128], [[0, 4], [1, 128]]), [kT_all, V_all], [maskT]))
                                hd0 = g * 8 + half * 4
                                sink_ap = None if isb else esrow[64:65, hd0:hd0 + 4, :].rearrange("p h t -> p (h t)")
                                attend((qT[gs, half * 512:(half + 1) * 512], [qT]), 512, kts, g, sink_ap,
                                       (attnT[0:64, hd0:hd0 + 4, :].rearrange("p h t -> p (h t)"), [attnT]))
                        for dh in range(2):
                            for hd in range(16):
                                k.op("pe", [attnT, wo_sb], [psM], lambda e, hd=hd: e.matmul(psM[:, :], attnT[0:64, hd, :], wo_sb[0:64, hd, dh * 512:(dh + 1) * 512], start=(hd == 0), stop=(hd == 15)))
                            k.op("dve", [h, psM], [z], lambda e: e.scalar_tensor_tensor(out=z[:, dh * 512:(dh + 1) * 512], in0=h[:, dh * 512:(dh + 1) * 512], scalar=ALPHA,
                                                                                        in1=psM[:, :], op0=ALU.mult, op1=ALU.add))
                        epi.run(z, ti)
                        blk += 1
                    for g in range(2):
                        gs = slice(g * 64, (g + 1) * 64)
                        kts = [(kTm[gs, 0:16], Vm[0:16, g * 65:(g + 1) * 65], 16, apx(mmeta[:, :], [[0, 8], [1, 16]]), [kTm, Vm], [mmeta])]
                        sink_ap = None if isb else esrow[64:65, g * 8:(g + 1) * 8, 0:16]
                        attend((qTm[gs, :, 32 * b:32 * b + 16], [qTm]), 128, kts, g, sink_ap,
                               (attnT_m[0:64, g * 8:(g + 1) * 8, 32 * b:32 * b + 16], [attnT_m]))
            h, z = hs[blk % 2], zs[blk % 2]
            k.dma("sp", h[:], H[MT * 128:(MT + 1) * 128, :], [], [h], h)
            k.op("dve", [], [z], lambda e: e.memset(z[:], 0.0))
            for dh in range(2):
                for hd in range(16):
                    k.op("pe", [attnT_m, wo_sb], [psM], lambda e, hd=hd: e.matmul(psM[0:48, :], attnT_m[0:64, hd, :], wo_sb[0:64, hd, dh * 512:(dh + 1) * 512], start=(hd == 0), stop=(hd == 15)))
                k.op("dve", [h, psM], [z], lambda e: e.scalar_tensor_tensor(out=z[0:48, dh * 512:(dh + 1) * 512], in0=h[0:48, dh * 512:(dh + 1) * 512], scalar=ALPHA,
                                                                            in1=psM[0:48, :], op0=ALU.mult, op1=ALU.add))
            epi.run(z, MT)

    class TBView:
        def __init__(self, base, ap):
            object.__setattr__(self, "base", base)
            object.__setattr__(self, "ap", ap)

        def __getitem__(self, idx):
            return self.ap[idx]

        def __getattr__(self, n):
            return getattr(self.base, n)

        def __setattr__(self, n, v):
            setattr(self.base, n, v)

    for p in phases:
        if p == "wc":
            phase_wcast()
        elif p == "p0":
            phase_p0()
        elif p == "m0":
            phase_moe(0)
        elif p == "m1":
            phase_moe(1)
        elif p == "a1":
            phase_proj("a")
        elif p == "b1":
            phase_proj("b")
        elif p == "a2":
            phase_attn("a", 0)
        elif p == "b2":
            phase_attn("b", 2)
    k.barrier()
    k.es.close()
    return nc


def _rope_tables(pos, rot_dim, head_dim, reps):
    half = rot_dim // 2
    inv = ROPE_THETA ** (-np.arange(half, dtype=np.float32) / half)
    ang = pos.astype(np.float32)[None, :] * inv[:, None]
    cos, sin = np.cos(ang).astype(np.float32), np.sin(ang).astype(np.float32)
    C = np.ones((head_dim, pos.shape[0]), np.float32)
    Sg = np.zeros((head_dim, pos.shape[0]), np.float32)
    C[:half] = cos
    C[half:rot_dim] = cos
    Sg[:half] = -sin
    Sg[half:rot_dim] = sin
    return np.tile(C, (reps, 1)), np.tile(Sg, (reps, 1)), cos.T.copy(), sin.T.copy()


def _swap_cols(w, head_dim, rot_dim):
    half = rot_dim // 2
    n = w.shape[-1] // head_dim
    idx = np.arange(w.shape[-1]).reshape(n, head_dim).copy()
    a = idx[:, :half].copy()
    idx[:, :half] = idx[:, half:rot_dim]
    idx[:, half:rot_dim] = a
    return w[..., idx.reshape(-1)]


def _pair_cols(w, npair, head_dim):
    n = w.shape[-1] // head_dim
    idx = np.arange(w.shape[-1]).reshape(n, head_dim)
    order = np.stack([idx[:npair], idx[npair:]], 1).reshape(-1)
    return w[..., order]


def prep_shared(inp, S, NE):
    NB = S // 128
    NT = 2 * NB + 1
    R = NT * 128
    f = lambda a: np.ascontiguousarray(np.asarray(a, dtype=np.float32))
    pos = np.zeros(R, np.int64)
    for b in range(2):
        pos[b * S:(b + 1) * S] = N_META + np.arange(S)
        pos[2 * S + 32 * b:2 * S + 32 * b + 16] = np.arange(16)
    cq, sq, _, _ = _rope_tables(pos, 16, 64, 2)
    ci, si, ckt, skt = _rope_tables(pos, 32, 64, 2)
    sh = {"cq": cq, "sq": sq, "ci": ci, "si": si, "ckt": ckt, "skt": skt}
    ii = np.arange(128)
    sh["mcur"] = (ii[:, None] <= ii[None, :]).astype(np.float32)
    sh["mprev"] = (ii[:, None] > ii[None, :]).astype(np.float32)
    sh["mmeta"] = (np.arange(16)[:, None] <= np.arange(16)[None, :]).astype(np.float32)
    sh["negu"] = np.where(ii[None, :] > ii[:, None], NEGM, 0.0).astype(np.float32)
    sh["ident"] = np.eye(128, dtype=np.float32)
    sh["lng"] = f(np.stack([inp["ln_mix_g"][0], inp["ln_ffn_g"][0], inp["ln_mix_g"][1], inp["ln_ffn_g"][1]]))
    sh["lnb"] = f(np.stack([inp["ln_mix_b"][0], inp["ln_ffn_b"][0], inp["ln_mix_b"][1], inp["ln_ffn_b"][1]]))
    for L, wi, bi, wo in (("a", inp["w_in_a"][0], inp["b_in_a"][0], inp["w_out_a"][0]),
                          ("b", inp["w_in_b"][0], inp["b_in_b"][0], inp["w_out_b"][0])):
        wi = np.asarray(wi, np.float32)
        bi = np.asarray(bi, np.float32)
        wq, bq = wi[:, :1024], bi[:1024]
        wk, bk = wi[:, 1024:1152], bi[1024:1152]
        sh["wq_" + L] = f(_pair_cols(wq, 8, 64))
        sh["wqs_" + L] = f(_pair_cols(_swap_cols(wq, 64, 16), 8, 64))
        sh["bq_" + L] = f(_pair_cols(bq, 8, 64).reshape(8, 128).T)
        sh["bqs_" + L] = f(_pair_cols(_swap_cols(bq, 64, 16), 8, 64).reshape(8, 128).T)
        sh["wk_" + L] = f(wk)
        sh["wks_" + L] = f(_swap_cols(wk, 64, 16))
        sh["bk_" + L] = f(bk.reshape(128, 1))
        sh["bks_" + L] = f(_swap_cols(bk, 64, 16).reshape(128, 1))
        sh["wo_" + L] = f(wo)
    wa, ba = np.asarray(inp["w_in_a"][0], np.float32), np.asarray(inp["b_in_a"][0], np.float32)
    wb, bb = np.asarray(inp["w_in_b"][0], np.float32), np.asarray(inp["b_in_b"][0], np.float32)
    sh["wvx_a"] = f(wa[:, 1152:1280])
    sh["bvx_a"] = f(ba[1152:1280].reshape(1, 128))
    sh["wvx_b"] = f(np.concatenate([wb[:, 1152:1280], wb[:, 1792:1856], wb[:, 1856:1864]], 1))
    sh["bvx_b"] = f(np.concatenate([bb[1152:1280], bb[1792:1856], bb[1856:1864]]).reshape(1, 200))
    wqi, bqi = wb[:, 1280:1792], bb[1280:1792]
    sh["wqi"] = f(_pair_cols(wqi, 4, 64))
    sh["wqis"] = f(_pair_cols(_swap_cols(wqi, 64, 32), 4, 64))
    sh["bqi"] = f(_pair_cols(bqi, 4, 64).reshape(4, 128).T)
    sh["bqis"] = f(_pair_cols(_swap_cols(bqi, 64, 32), 4, 64).reshape(4, 128).T)
    sh["sinks"] = f(np.asarray(inp["sinks_a"][0]).reshape(1, 16))
    sh["idxg"] = f(np.asarray(inp["idx_k_norm_g"][0]).reshape(1, 64))
    sh["idxb"] = f(np.asarray(inp["idx_k_norm_b"][0]).reshape(1, 64))
    sh["wr"] = f(inp["w_router"])
    sh["br"] = f(inp["b_router"])
    for l in (0, 1):
        sh["wgu%d" % l] = f(inp["w_gate_up"][l]).reshape(NE * D, 2048)
        sh["wd%d" % l] = f(inp["w_down"][l]).reshape(NE * D, D)
    sh["bgu"] = f(np.asarray(inp["b_gate_up"], np.float32).reshape(2, NE, 16, 128).transpose(0, 3, 1, 2).reshape(2, 128, NE * 16))
    sh["bd"] = f(inp["b_down"])
    return sh


_NC_CACHE = {}
LAUNCHES = (("wc", "p0", "a1", "a2", "m0", "b1", "b2", "m1"),)


def _get_nc(S, NE, phases):
    key = (S, NE, tuple(phases))
    if key not in _NC_CACHE:
        _NC_CACHE[key] = build(S, NE, tuple(phases))
    return _NC_CACHE[key]


def run(inp, launches=LAUNCHES, trace=False):
    x = np.asarray(inp["x"], np.float32)
    Bt, S, _ = x.shape
    NE = inp["w_router"].shape[-1]
    assert Bt == 16
    NT = 2 * (S // 128) + 1
    R = NT * 128
    sh = prep_shared(inp, S, NE)
    meta = np.asarray(inp["meta_tokens"], np.float32)
    hins = []
    for c in range(8):
        h = np.zeros((R, D), np.float32)
        h[:2 * S] = x[2 * c:2 * c + 2].reshape(2 * S, D)
        h[2 * S:2 * S + 16] = meta
        h[2 * S + 32:2 * S + 48] = meta
        hins.append(h)
    res = None
    for phases in launches:
        nc = _get_nc(S, NE, phases)
        moe_l = [l for l in (0, 1) if ("m%d" % l) in phases]
        skip = set()
        for l in (0, 1):
            if l not in moe_l:
                skip.add("wgu%d" % l)
                skip.add("wd%d" % l)
        in_maps = []
        for c in range(8):
            m = {kk: v for kk, v in sh.items() if kk not in skip}
            m["hin"] = hins[c]
            in_maps.append(m)
        res = run_bass_kernel_spmd(nc, in_maps, core_ids=list(range(8)), **({"trace": True} if trace else {}))
        hins = [np.ascontiguousarray(np.asarray(r["out"], dtype=np.float32)) for r in res.results]
    return hins, res


def kernel(**inputs):
    outs, _ = run(inputs)
    S = inputs["x"].shape[1]
    y = np.concatenate([o[:2 * S].reshape(2, S, D) for o in outs], 0)
    return y.astype(np.float32, copy=False)
```

```python
import numpy as np
from contextlib import ExitStack
import ml_dtypes
import concourse.bass as bass
import concourse.mybir as mybir
from concourse.bass_utils import run_bass_kernel_spmd

F32 = mybir.dt.float32
BF16 = mybir.dt.bfloat16
AF = mybir.ActivationFunctionType
ALU = mybir.AluOpType
AX = mybir.AxisListType

D = 1024
N_META = 16
HD = 64
NH = 16
ROPE_THETA = 500000.0
ATTN_SCALE = HD ** -0.5
IDX_HEADS = 8
IDX_SCALE = 64 ** -0.5
IDX_W_SCALE = IDX_HEADS ** -0.5
TOPK_MAX = 256
SWIGLU_LIMIT = 7.0
SWIGLU_ALPHA = 1.702
DEPTH = 2
ALPHA = (2 * DEPTH) ** 0.25
LN_EPS = 1e-5
NEGM = -30000.0
N_BISECT = 20


class TB:
    __slots__ = ("t", "w", "r", "dsem", "dcnt")

    def __init__(self, t):
        self.t = t
        self.w = {}
        self.r = {}
        self.dsem = None
        self.dcnt = 0

    def __getitem__(self, idx):
        return self.t[idx]


class KC:
    def __init__(self, nc):
        self.nc = nc
        self.es = ExitStack()
        self.sem_max = {}
        self.sems = {}
        self.eng = {}
        for name, e in (("pe", nc.tensor), ("dve", nc.vector), ("act", nc.scalar),
                        ("pool", nc.gpsimd), ("sp", nc.sync)):
            s = self.es.enter_context(nc.semaphore("s_" + name))
            self.eng[name] = [e, s, 0, {}]
        self.nsem = 5
        self.uid = 0
        self.pools = {}
        self.pool_idx = {}

    def newsem(self, name):
        self.nsem += 1
        assert self.nsem < 140, "too many semaphores"
        return self.es.enter_context(self.nc.semaphore(name))

    def _deps(self, reads, writes):
        raw = {}
        oth = {}
        for b in reads:
            for s, v in b.w.items():
                if raw.get(s, 0) < v:
                    raw[s] = v
        for b in writes:
            for dd in (b.w, b.r):
                for s, v in dd.items():
                    if oth.get(s, 0) < v:
                        oth[s] = v
        return raw, oth

    def _wait(self, ename, deps):
        E = self.eng[ename]
        raw, oth = deps
        need = dict(raw)
        for s, v in oth.items():
            if need.get(s, 0) < v:
                need[s] = v
        for s, v in need.items():
            if ename == "pe" and s is E[1]:
                continue
            if E[3].get(s, 0) < v:
                E[0].wait_ge(s, v)
                E[3][s] = v

    def _commit(self, reads, writes, s, v):
        for b in writes:
            b.w = {s: v}
            b.r = {}
        for b in reads:
            if b.r.get(s, 0) < v:
                b.r[s] = v
        self.sem_max[s] = v

    def op(self, ename, reads, writes, fn):
        E = self.eng[ename]
        self._wait(ename, self._deps(reads, writes))
        ins = fn(E[0])
        E[2] += 1
        ins.then_inc(E[1], 1)
        self._commit(reads, writes, E[1], E[2])

    def dma(self, ename, out, in_, reads, writes, sb, **kw):
        E = self.eng[ename]
        self._wait(ename, self._deps(reads, writes))
        if sb.dsem is None:
            sb.dsem = {}
        if ename not in sb.dsem:
            pool = self.pools.setdefault(ename, [])
            idx = self.pool_idx.get(ename, 0)
            self.pool_idx[ename] = idx + 1
            if idx >= len(pool) and len(pool) < 44:
                self.uid += 1
                pool.append([self.newsem("d%d" % self.uid), 0])
            sb.dsem[ename] = pool[idx % len(pool)]
        ds = sb.dsem[ename]
        ins = E[0].dma_start(out=out, in_=in_, **kw)
        ds[1] += 16
        ins.then_inc(ds[0], 16)
        self._commit(reads, writes, ds[0], ds[1])

    def barrier(self):
        for ename, E in self.eng.items():
            for s, v in self.sem_max.items():
                if E[3].get(s, 0) < v:
                    E[0].wait_ge(s, v)
                    E[3][s] = v


class Phase:
    def __init__(self, k):
        self.k = k
        self.es = ExitStack()
        self.n = 0

    def __enter__(self):
        self.es.__enter__()
        self.k.pool_idx = {}
        return self

    def __exit__(self, *a):
        self.k.barrier()
        return self.es.__exit__(*a)

    def sb(self, shape, dt, name=None):
        self.n += 1
        self.k.uid += 1
        return TB(self.es.enter_context(self.k.nc.sbuf_tensor("%s_%d" % (name or "sb", self.k.uid), list(shape), dt)))

    def ps(self, shape, dt, name=None):
        self.k.uid += 1
        return TB(self.es.enter_context(self.k.nc.psum_tensor("%s_%d" % (name or "ps", self.k.uid), list(shape), dt)))


def apx(ap, pat, off=0):
    base = list(ap.ap)
    return bass.AP(tensor=ap.tensor, offset=ap.offset + off, ap=[list(base[0])] + [list(p) for p in pat])


def dram_bcast(ap_row, nparts):
    base = list(ap_row.ap)
    return bass.AP(tensor=ap_row.tensor, offset=ap_row.offset, ap=[[0, nparts]] + [list(p) for p in base[-1:]])


def build(S, NE, phases=("p0", "a1", "a2", "m0", "b1", "b2", "m1")):
    NB = S // 128
    NT = 2 * NB + 1
    R = NT * 128
    MT = NT - 1
    TOPK = min(TOPK_MAX, S // 4)
    nc = bass.Bass("TRN2", target_bir_lowering=False)

    def din(name, shape, dt=F32):
        return nc.dram_tensor(name, list(shape), dt, kind="ExternalInput").ap()

    def dscr(name, shape, dt):
        return nc.dram_tensor(name, list(shape), dt, kind="Internal").ap()

    hin = din("hin", [R, D])
    H = nc.dram_tensor("out", [R, D], F32, kind="ExternalOutput").ap()
    HT = dscr("HT", [D, R], BF16)
    lng = din("lng", [4, D])
    lnb = din("lnb", [4, D])
    ident_d = din("ident", [128, 128])
    W = {}
    for L, nq in (("a", 8), ("b", 8)):
        W["wq_" + L] = din("wq_" + L, [D, 1024])
        W["wqs_" + L] = din("wqs_" + L, [D, 1024])
        W["bq_" + L] = din("bq_" + L, [128, 8])
        W["bqs_" + L] = din("bqs_" + L, [128, 8])
        W["wk_" + L] = din("wk_" + L, [D, 128])
        W["wks_" + L] = din("wks_" + L, [D, 128])
        W["bk_" + L] = din("bk_" + L, [128, 1])
        W["bks_" + L] = din("bks_" + L, [128, 1])
        W["wo_" + L] = din("wo_" + L, [D, D])
    W["wvx_a"] = din("wvx_a", [D, 128])
    W["bvx_a"] = din("bvx_a", [1, 128])
    W["wvx_b"] = din("wvx_b", [D, 200])
    W["bvx_b"] = din("bvx_b", [1, 200])
    W["wqi"] = din("wqi", [D, 512])
    W["wqis"] = din("wqis", [D, 512])
    W["bqi"] = din("bqi", [128, 4])
    W["bqis"] = din("bqis", [128, 4])
    sinks = din("sinks", [1, 16])
    idxg = din("idxg", [1, 64])
    idxb = din("idxb", [1, 64])
    wr = din("wr", [2, D, NE])
    br = din("br", [2, NE])
    moe_layers = [l for l in (0, 1) if ("m%d" % l) in phases]
    wgu_l = {l: din("wgu%d" % l, [NE * D, 2048]) for l in moe_layers}
    bgu = din("bgu", [2, 128, NE * 16])
    wd_l = {l: din("wd%d" % l, [NE * D, D]) for l in moe_layers}
    bd = din("bd", [2, NE, D])
    cq = din("cq", [128, R])
    sq = din("sq", [128, R])
    ci = din("ci", [128, R])
    si = din("si", [128, R])
    ckt = din("ckt", [R, 16])
    skt = din("skt", [R, 16])
    mcur_d = din("mcur", [128, 128])
    mprev_d = din("mprev", [128, 128])
    mmeta_d = din("mmeta", [16, 16])
    negu_d = din("negu", [128, 128])
    QT = dscr("QT", [NT, 128, 1024], BF16)
    KT = dscr("KT", [128, R], BF16)
    VA = dscr("VA", [R, 130], BF16)
    QIT = dscr("QIT", [NT, 128, 512], BF16)
    KIT = dscr("KIT", [128, R], BF16)
    WI = dscr("WI", [R, 8], F32)

    WGB = {l: dscr("WGB%d" % l, [NE * D, 2048], BF16) for l in moe_layers}
    WDB = {l: dscr("WDB%d" % l, [NE * D, D], BF16) for l in moe_layers}

    k = KC(nc)

    def phase_wcast():
        if True:
            owners = [TB(None) for _ in range(4)]
            i = 0
            for l in moe_layers:
                for r0 in range(0, NE * D, 128):
                    o = owners[i % 4]
                    i += 1
                    k.dma("pool", WGB[l][r0:r0 + 128, :], wgu_l[l][r0:r0 + 128, :], [], [], o)
                    k.dma("pool", WDB[l][r0:r0 + 128, :], wd_l[l][r0:r0 + 128, :], [], [], o)

    class Epi:
        def __init__(self, ph, lnidx, nb=2, stq="pool"):
            self.ph = ph
            self.nb = nb
            self.stq = stq
            self.ln = lnidx is not None
            if self.ln:
                self.g = ph.sb([128, D], F32, "lng")
                self.b = ph.sb([128, D], F32, "lnb")
                k.dma("sp", self.g[:], dram_bcast(lng[lnidx:lnidx + 1, :], 128), [], [self.g], self.g)
                k.dma("sp", self.b[:], dram_bcast(lnb[lnidx:lnidx + 1, :], 128), [], [self.b], self.b)
            self.idb = ph.sb([128, 128], BF16, "idb")
            if stq == "pool":
                k.dma("pool", self.idb[:], ident_d[:, :], [], [self.idb], self.idb)
            else:
                id32 = ph.sb([128, 128], F32, "id32")
                k.dma("sp", id32[:], ident_d[:, :], [], [id32], id32)
                k.op("act", [id32], [self.idb], lambda e: e.activation(out=self.idb[:], in_=id32[:], func=AF.Copy))
            self.st = [ph.sb([128, 2, 6], F32, "bnst") for _ in range(nb)]
            self.mv = [ph.sb([128, 4], F32, "mv") for _ in range(nb)]
            self.o = [ph.sb([128, D], F32, "epo") for _ in range(nb)]
            self.ob = [ph.sb([128, D], BF16, "epob") for _ in range(nb)]
            self.tt = [ph.sb([128, D], BF16, "eptt") for _ in range(nb)]
            self.pt = [ph.ps([128, D], BF16, "eppt") for _ in range(1)]
            self.i = 0

        def run(self, z, ti, eng2="pool"):
            i = self.i
            self.i += 1
            nb = self.nb
            st, mv, o, ob, tt = self.st[i % nb], self.mv[i % nb], self.o[i % nb], self.ob[i % nb], self.tt[i % nb]
            pt = self.pt[0]
            if self.ln:
                for c in range(2):
                    k.op("dve", [z], [st], lambda e, c=c: e.bn_stats(st[:, c, :], z[:, c * 512:(c + 1) * 512]))
                k.op("dve", [st], [mv], lambda e: e.bn_aggr(mv[:, 0:2], st[:]))
                k.op("dve", [mv], [mv], lambda e: e.tensor_scalar(out=mv[:, 2:3], in0=mv[:, 1:2], scalar1=LN_EPS,
                                                                  scalar2=None, op0=ALU.add))
                k.op("act", [mv], [mv], lambda e: e.activation(out=mv[:, 3:4], in_=mv[:, 2:3], func=AF.Sqrt))
                k.op("dve", [mv], [mv], lambda e: e.reciprocal(out=mv[:, 2:3], in_=mv[:, 3:4]))
                k.op("dve", [z, mv], [o], lambda e: e.tensor_scalar(out=o[:], in0=z[:], scalar1=mv[:, 0:1],
                                                                    scalar2=mv[:, 2:3], op0=ALU.subtract, op1=ALU.mult))
                k.op(eng2, [o, self.g], [o], lambda e: e.tensor_tensor(out=o[:], in0=o[:], in1=self.g[:], op=ALU.mult))
                k.op(eng2, [o, self.b], [o], lambda e: e.tensor_tensor(out=o[:], in0=o[:], in1=self.b[:], op=ALU.add))
                src = o
            else:
                src = z
            k.dma(self.stq, H[ti * 128:(ti + 1) * 128, :], src[:], [src], [], src)
            k.op("act", [src], [ob], lambda e: e.activation(out=ob[:], in_=src[:], func=AF.Copy))
            for c in range(8):
                k.op("pe", [ob, self.idb], [pt], lambda e, c=c: e.transpose(pt[:, c * 128:(c + 1) * 128], ob[:, c * 128:(c + 1) * 128], self.idb[:]))
            k.op("act", [pt], [tt], lambda e: e.activation(out=tt[:], in_=pt[:], func=AF.Copy))
            k.dma(self.stq, HT.rearrange("(c p) n -> p c n", p=128)[:, :, ti * 128:(ti + 1) * 128],
                  tt[:].rearrange("p (c n) -> p c n", c=8), [tt], [], tt)

    def phase_p0():
        with Phase(k) as ph:
            epi = Epi(ph, None, stq="sp")
            zs = [ph.sb([128, D], F32, "z0") for _ in range(2)]
            for ti in range(NT):
                z = zs[ti % 2]
                k.dma("sp", z[:], hin[ti * 128:(ti + 1) * 128, :], [], [z], z)
                epi.run(z, ti)

    def phase_moe(l):
        with Phase(k) as ph:
            import os
            epi = Epi(ph, None if os.environ.get("KDEBUG_NOLN") else 2 * l + 1, nb=int(os.environ.get("KDEBUG_NB", "1")))
            CW = 1024 if NT > 9 else 512
            TPC = CW // 128
            wr_sb = ph.sb([128, 8, NE], BF16, "wr")
            k.dma("pool", wr_sb[:], wr[l].rearrange("(c p) e -> p c e", p=128), [], [wr_sb], wr_sb)
            br_sb = ph.sb([128, NE], F32, "br")
            k.dma("sp", br_sb[:], dram_bcast(br[l:l + 1, :], 128), [], [br_sb], br_sb)
            bd_sb = ph.sb([32, D], BF16, "bd")
            k.dma("pool", bd_sb[0:NE, :], bd[l], [], [bd_sb], bd_sb)
            bgu_sb = ph.sb([128, NE * 16], F32, "bgu")
            k.dma("sp", bgu_sb[:], bgu[l], [], [bgu_sb], bgu_sb)
            ident = ph.sb([128, 128], F32, "ident")
            k.dma("sp", ident[:], ident_d[:, :], [], [ident], ident)
            hT = ph.sb([128, 8, CW], BF16, "hT")
            acc = ph.sb([128, TPC, D], F32, "acc")
            G = ph.sb([128, TPC, NE], F32, "G")
            GT = ph.sb([32, TPC, 128], BF16, "GT")
            lg = ph.sb([128, NE], F32, "lg")
            m8 = ph.sb([128, 8], F32, "m8")
            sm = ph.sb([128, 4], F32, "sm")
            wg_sb = [ph.sb([128, 8, 2048], BF16, "wg") for _ in range(2)]
            wd_sb = [ph.sb([128, 8, D], BF16, "wd") for _ in range(2)]
            actT = [ph.sb([128, 8, 512], BF16, "actT") for _ in range(2)]
            gc = [ph.sb([128, 512], F32, "gc") for _ in range(3)]
            sg = [ph.sb([128, 512], F32, "sg") for _ in range(3)]
            uc = [ph.sb([128, 512], F32, "uc") for _ in range(3)]
            psg = [ph.ps([128, 512], F32, "psg") for _ in range(2)]
            psu = [ph.ps([128, 512], F32, "psu") for _ in range(2)]
            psy = [ph.ps([128, 512], F32, "psy") for _ in range(2)]
            psr = ph.ps([128, 512], F32, "psr")
            wcnt = 0
            cnt_gu = 0
            cnt_y = 0
            nchunks = (NT * 128 + CW - 1) // CW
            for ch in range(nchunks):
                r0 = ch * CW
                cw = min(CW, NT * 128 - r0)
                tpc = cw // 128
                k.dma("sp", hT[:, :, 0:cw], HT.rearrange("(c p) n -> p c n", p=128)[:, :, r0:r0 + cw], [], [hT], hT)
                k.dma("sp", acc[:, 0:tpc, :], H[r0:r0 + cw, :].rearrange("(t p) d -> p t d", p=128), [], [acc], acc)
                for t in range(tpc):
                    for c in range(8):
                        k.op("pe", [hT, wr_sb], [psr], lambda e, c=c, t=t: e.matmul(
                            psr[:, 0:NE], hT[:, c, t * 128:(t + 1) * 128], wr_sb[:, c, :], start=(c == 0), stop=(c == 7)))
                    k.op("dve", [psr, br_sb], [lg], lambda e: e.tensor_tensor(out=lg[:], in0=psr[:, 0:NE], in1=br_sb[:], op=ALU.add))
                    k.op("dve", [lg], [m8], lambda e: e.max(out=m8[:], in_=lg[:]))
                    k.op("dve", [m8], [sm], lambda e: e.tensor_scalar(out=sm[:, 0:1], in0=m8[:, 0:1], scalar1=-1.0, scalar2=None, op0=ALU.mult))
                    k.op("act", [lg, sm], [G], lambda e, t=t: e.activation(out=G[:, t, :], in_=lg[:], func=AF.Exp, bias=sm[:, 0:1], scale=1.0))
                    k.op("dve", [lg, m8], [lg], lambda e: e.tensor_scalar(out=lg[:], in0=lg[:], scalar1=m8[:, 3:4], scalar2=None, op0=ALU.is_ge))
                    k.op("dve", [G, lg], [G], lambda e, t=t: e.tensor_tensor(out=G[:, t, :], in0=G[:, t, :], in1=lg[:], op=ALU.mult))
                    k.op("dve", [G], [sm], lambda e, t=t: e.tensor_reduce(out=sm[:, 1:2], in_=G[:, t, :], axis=AX.X, op=ALU.add))
                    k.op("dve", [sm], [sm], lambda e: e.reciprocal(out=sm[:, 2:3], in_=sm[:, 1:2]))
                    k.op("dve", [G, sm], [G], lambda e, t=t: e.tensor_scalar(out=G[:, t, :], in0=G[:, t, :], scalar1=sm[:, 2:3], scalar2=None, op0=ALU.mult))
                    k.op("pe", [G, ident], [psr], lambda e, t=t: e.matmul(psr[0:NE, 128:256], G[:, t, :], ident[:], start=True, stop=True))
                    k.op("act", [psr], [GT], lambda e, t=t: e.activation(out=GT[0:NE, t, :], in_=psr[0:NE, 128:256], func=AF.Copy))
                    for dh in range(2):
                        py = psy[cnt_y % 2]
                        cnt_y += 1
                        k.op("pe", [GT, bd_sb], [py], lambda e, t=t, dh=dh, py=py: e.matmul(
                            py[:], GT[0:NE, t, :], bd_sb[0:NE, dh * 512:(dh + 1) * 512], start=True, stop=True))
                        k.op("dve", [py, acc], [acc], lambda e, t=t, dh=dh, py=py: e.scalar_tensor_tensor(
                            out=acc[:, t, dh * 512:(dh + 1) * 512], in0=acc[:, t, dh * 512:(dh + 1) * 512], scalar=ALPHA,
                            in1=py[:], op0=ALU.mult, op1=ALU.add))
                for ex in range(NE):
                    wg_t = wg_sb[wcnt % 2]
                    wd_t = wd_sb[wcnt % 2]
                    wcnt += 1
                    e0 = ex * D
                    wgu = wgu_l[l]
                    wd = wd_l[l]
                    for hh in range(2):
                        k.dma("sp", wg_t[:, hh * 4:(hh + 1) * 4, :],
                              WGB[l][e0 + hh * 512:e0 + (hh + 1) * 512, :].rearrange("(c p) f -> p c f", p=128), [], [wg_t], wg_t)
                    k.dma("sp", wd_t[:], WDB[l][e0:e0 + D, :].rearrange("(c p) f -> p c f", p=128), [], [wd_t], wd_t)
                    for hf in range((cw + 511) // 512):
                        c0 = hf * 512
                        w = min(512, cw - c0)
                        aT = actT[(cnt_gu // 8) % 2]
                        for fc in range(8):
                            pg = psg[cnt_gu % 2]
                            pu = psu[cnt_gu % 2]
                            g_ = gc[cnt_gu % 3]
                            s_ = sg[cnt_gu % 3]
                            u_ = uc[cnt_gu % 3]
                            cnt_gu += 1
                            for c in range(8):
                                k.op("pe", [wg_t, hT], [pg], lambda e, c=c, fc=fc, pg=pg: e.matmul(
                                    pg[:, 0:w], wg_t[:, c, fc * 128:(fc + 1) * 128], hT[:, c, c0:c0 + w], start=(c == 0), stop=(c == 7)))
                            for c in range(8):
                                k.op("pe", [wg_t, hT], [pu], lambda e, c=c, fc=fc, pu=pu: e.matmul(
                                    pu[:, 0:w], wg_t[:, c, 1024 + fc * 128:1024 + (fc + 1) * 128], hT[:, c, c0:c0 + w], start=(c == 0), stop=(c == 7)))
                            bcol = ex * 16 + fc
                            k.op("act", [pu, bgu_sb], [u_], lambda e, pu=pu, u_=u_, bcol=bcol: e.activation(
                                out=u_[:, 0:w], in_=pu[:, 0:w], func=AF.Identity, bias=bgu_sb[:, bcol + 8:bcol + 9], scale=1.0))
                            k.op("dve", [pg, bgu_sb], [g_], lambda e, pg=pg, g_=g_, bcol=bcol: e.tensor_scalar(
                                out=g_[:, 0:w], in0=pg[:, 0:w], scalar1=bgu_sb[:, bcol:bcol + 1], scalar2=SWIGLU_LIMIT, op0=ALU.add, op1=ALU.min))
                            k.op("act", [g_], [s_], lambda e, g_=g_, s_=s_: e.activation(out=s_[:, 0:w], in_=g_[:, 0:w], func=AF.Sigmoid, scale=SWIGLU_ALPHA))
                            k.op("dve", [u_], [u_], lambda e, u_=u_: e.tensor_scalar(
                                out=u_[:, 0:w], in0=u_[:, 0:w], scalar1=-SWIGLU_LIMIT, scalar2=SWIGLU_LIMIT, op0=ALU.max, op1=ALU.min))
                            k.op("pool", [g_, s_], [g_], lambda e, g_=g_, s_=s_: e.tensor_tensor(out=g_[:, 0:w], in0=g_[:, 0:w], in1=s_[:, 0:w], op=ALU.mult))
                            k.op("dve", [g_, u_], [aT], lambda e, g_=g_, u_=u_, aT=aT, fc=fc: e.scalar_tensor_tensor(
                                out=aT[:, fc, 0:w], in0=u_[:, 0:w], scalar=1.0, in1=g_[:, 0:w], op0=ALU.add, op1=ALU.mult))
                        for t in range(w // 128):
                            tt_ = hf * 4 + t
                            for dh in range(2):
                                py = psy[cnt_y % 2]
                                cnt_y += 1
                                for fc in range(8):
                                    k.op("pe", [aT, wd_t], [py], lambda e, fc=fc, t=t, dh=dh, py=py, aT=aT: e.matmul(
                                        py[:], aT[:, fc, t * 128:(t + 1) * 128], wd_t[:, fc, dh * 512:(dh + 1) * 512], start=(fc == 0), stop=(fc == 7)))
                                k.op("dve", [py, G, acc], [acc], lambda e, tt_=tt_, dh=dh, py=py, ex=ex: e.scalar_tensor_tensor(
                                    out=acc[:, tt_, dh * 512:(dh + 1) * 512], in0=py[:], scalar=G[:, tt_, ex:ex + 1],
                                    in1=acc[:, tt_, dh * 512:(dh + 1) * 512], op0=ALU.mult, op1=ALU.add))
                for t in range(tpc):
                    zview = TBView(acc, acc[:, t, :])
                    epi.run(zview, r0 // 128 + t)


    def phase_proj(L):
        isb = (L == "b")
        NV = 200 if isb else 128
        with Phase(k) as ph:
            def wload(src, shape, pat, **kw):
                t = ph.sb(shape, BF16, "w")
                k.dma("pool", t[:], src.rearrange(pat, **kw), [], [t], t)
                return t

            def fload(src, shape):
                t = ph.sb(shape, F32, "c")
                k.dma("sp", t[:], src, [], [t], t)
                return t
            wq = wload(W["wq_" + L], [128, 8, 1024], "(c p) n -> p c n", p=128)
            wqs = wload(W["wqs_" + L], [128, 8, 1024], "(c p) n -> p c n", p=128)
            wk = wload(W["wk_" + L], [128, 8, 128], "(c p) n -> p c n", p=128)
            wks = wload(W["wks_" + L], [128, 8, 128], "(c p) n -> p c n", p=128)
            wvx = wload(W["wvx_" + L], [128, 8, NV], "(c p) n -> p c n", p=128)
            bq = fload(W["bq_" + L][:, :], [128, 8])
            bqs = fload(W["bqs_" + L][:, :], [128, 8])
            bk = fload(W["bk_" + L][:, :], [128, 1])
            bks = fload(W["bks_" + L][:, :], [128, 1])
            bvx = fload(dram_bcast(W["bvx_" + L][0:1, :], 128), [128, NV])
            if isb:
                wqi = wload(W["wqi"], [128, 8, 512], "(c p) n -> p c n", p=128)
                wqis = wload(W["wqis"], [128, 8, 512], "(c p) n -> p c n", p=128)
                bqi = fload(W["bqi"][:, :], [128, 4])
                bqis = fload(W["bqis"][:, :], [128, 4])
                ig = fload(dram_bcast(idxg[0:1, :], 128), [128, 64])
                ib = fload(dram_bcast(idxb[0:1, :], 128), [128, 64])
                ident = fload(ident_d[:, :], [128, 128])
            hTs = [ph.sb([128, 8, 512], BF16, "hT") for _ in range(2)]
            cts = [ph.sb([128, 512], F32, "ct") for _ in range(2)]
            sts = [ph.sb([128, 512], F32, "st") for _ in range(2)]
            qTs = [ph.sb([128, 8, 512], BF16, "qT") for _ in range(2)]
            kTs = [ph.sb([128, 512], BF16, "kT") for _ in range(2)]
            t1s = [ph.sb([128, 512], F32, "t1") for _ in range(2)]
            t2s = [ph.sb([128, 512], F32, "t2") for _ in range(2)]
            vxs = [ph.sb([128, NV], F32, "vx") for _ in range(2)]
            vas = [ph.sb([128, 130], BF16, "va") for _ in range(2)]
            for va in vas:
                k.op("dve", [], [va], lambda e, va=va: e.memset(va[:], 1.0))
            psA = [ph.ps([128, 512], F32, "psA") for _ in range(2)]
            psB = [ph.ps([128, 512], F32, "psB") for _ in range(2)]
            psV = [ph.ps([128, 512], F32, "psV") for _ in range(2)]
            if isb:
                cis = [ph.sb([128, 512], F32, "ci") for _ in range(2)]
                sis = [ph.sb([128, 512], F32, "si") for _ in range(2)]
                qiTs = [ph.sb([128, 4, 512], BF16, "qiT") for _ in range(2)]
                cks = [ph.sb([128, 16], F32, "ck") for _ in range(2)]
                sks = [ph.sb([128, 16], F32, "sk") for _ in range(2)]
                kns = [ph.sb([128, 64], F32, "kn") for _ in range(2)]
                kds = [ph.sb([128, 128], F32, "kd") for _ in range(2)]
                tmps = [ph.sb([128, 64], F32, "tmp") for _ in range(2)]
                kst = [ph.sb([128, 8], F32, "kst") for _ in range(2)]
                kiTs = [ph.sb([128, 128], BF16, "kiT") for _ in range(2)]
                wis = [ph.sb([128, 8], F32, "wis") for _ in range(2)]
                psT = ph.ps([128, 128], F32, "psT")
            chunks = [(r0, min(512, 2 * S - r0)) for r0 in range(0, 2 * S, 512)] + [(2 * S, 128)]
            cnt = 0
            vcnt = 0

            def rope_pair(wA, wB, bA, bB, j, hT, w, ct, st, outap, nout=128):
                nonlocal cnt
                pa, pb, t1, t2 = psA[cnt % 2], psB[cnt % 2], t1s[cnt % 2], t2s[cnt % 2]
                cnt += 1
                for c in range(8):
                    k.op("pe", [wA, hT], [pa], lambda e, c=c: e.matmul(pa[:, 0:w], wA[:, c, j * 128:(j + 1) * 128], hT[:, c, 0:w], start=(c == 0), stop=(c == 7)))
                for c in range(8):
                    k.op("pe", [wB, hT], [pb], lambda e, c=c: e.matmul(pb[:, 0:w], wB[:, c, j * 128:(j + 1) * 128], hT[:, c, 0:w], start=(c == 0), stop=(c == 7)))
                k.op("dve", [pa, bA, ct], [t1], lambda e: e.scalar_tensor_tensor(out=t1[:, 0:w], in0=pa[:, 0:w], scalar=bA[:, j:j + 1], in1=ct[:, 0:w], op0=ALU.add, op1=ALU.mult))
                k.op("dve", [pb, bB, st], [t2], lambda e: e.scalar_tensor_tensor(out=t2[:, 0:w], in0=pb[:, 0:w], scalar=bB[:, j:j + 1], in1=st[:, 0:w], op0=ALU.add, op1=ALU.mult))
                return t1, t2

            for ci_, (r0, w) in enumerate(chunks):
                hT, ct, st, qT, kT = hTs[ci_ % 2], cts[ci_ % 2], sts[ci_ % 2], qTs[ci_ % 2], kTs[ci_ % 2]
                k.dma("sp", hT[:, :, 0:w], HT.rearrange("(c p) n -> p c n", p=128)[:, :, r0:r0 + w], [], [hT], hT)
                k.dma("sp", ct[:, 0:w], cq[:, r0:r0 + w], [], [ct], ct)
                k.dma("sp", st[:, 0:w], sq[:, r0:r0 + w], [], [st], st)
                for j in range(8):
                    t1, t2 = rope_pair(wq, wqs, bq, bqs, j, hT, w, ct, st, None)
                    k.op("pool", [t1, t2], [qT], lambda e, j=j, t1=t1, t2=t2: e.tensor_tensor(out=qT[:, j, 0:w], in0=t1[:, 0:w], in1=t2[:, 0:w], op=ALU.add))
                for i in range(w // 128):
                    ti = r0 // 128 + i
                    k.dma("pool", QT[ti].rearrange("p (j t) -> p j t", j=8), qT[:, :, i * 128:(i + 1) * 128], [qT], [], qT)
                t1, t2 = rope_pair(wk, wks, bk, bks, 0, hT, w, ct, st, None)
                k.op("pool", [t1, t2], [kT], lambda e, t1=t1, t2=t2: e.tensor_tensor(out=kT[:, 0:w], in0=t1[:, 0:w], in1=t2[:, 0:w], op=ALU.add))
                k.dma("pool", KT[:, r0:r0 + w], kT[:, 0:w], [kT], [], kT)
                if isb:
                    cit, sit, qiT = cis[ci_ % 2], sis[ci_ % 2], qiTs[ci_ % 2]
                    k.dma("sp", cit[:, 0:w], ci[:, r0:r0 + w], [], [cit], cit)
                    k.dma("sp", sit[:, 0:w], si[:, r0:r0 + w], [], [sit], sit)
                    for j in range(4):
                        t1, t2 = rope_pair(wqi, wqis, bqi, bqis, j, hT, w, cit, sit, None)
                        k.op("pool", [t1, t2], [qiT], lambda e, j=j, t1=t1, t2=t2: e.tensor_tensor(out=qiT[:, j, 0:w], in0=t1[:, 0:w], in1=t2[:, 0:w], op=ALU.add))
                    for i in range(w // 128):
                        ti = r0 // 128 + i
                        k.dma("pool", QIT[ti].rearrange("p (j t) -> p j t", j=4), qiT[:, :, i * 128:(i + 1) * 128], [qiT], [], qiT)
                for i in range(w // 128):
                    ti = r0 // 128 + i
                    pv, vx, va = psV[vcnt % 2], vxs[vcnt % 2], vas[vcnt % 2]
                    for c in range(8):
                        k.op("pe", [hT, wvx], [pv], lambda e, c=c: e.matmul(pv[:, 0:NV], hT[:, c, i * 128:(i + 1) * 128], wvx[:, c, :], start=(c == 0), stop=(c == 7)))
                    k.op("dve", [pv, bvx], [vx], lambda e: e.tensor_tensor(out=vx[:], in0=pv[:, 0:NV], in1=bvx[:], op=ALU.add))
                    k.op("act", [vx], [va], lambda e: e.activation(out=va[:, 0:130].rearrange("p (g c) -> p g c", g=2)[:, :, 0:64],
                                                                   in_=vx[:, 0:128].rearrange("p (g c) -> p g c", g=2), func=AF.Copy))
                    k.dma("pool", VA[ti * 128:(ti + 1) * 128, :], va[:], [va], [], va)
                    if isb:
                        ck, sk, kn, kd, tmp, ks, kiT, wi_ = (cks[vcnt % 2], sks[vcnt % 2], kns[vcnt % 2], kds[vcnt % 2],
                                                               tmps[vcnt % 2], kst[vcnt % 2], kiTs[vcnt % 2], wis[vcnt % 2])
                        k.dma("sp", ck[:], ckt[ti * 128:(ti + 1) * 128, :], [], [ck], ck)
                        k.dma("sp", sk[:], skt[ti * 128:(ti + 1) * 128, :], [], [sk], sk)
                        k.op("dve", [vx], [tmp], lambda e: e.bn_stats(tmp[:, 0:6], vx[:, 128:192]))
                        k.op("dve", [tmp], [ks], lambda e: e.bn_aggr(ks[:, 0:2], tmp[:, 0:6]))
                        k.op("dve", [ks], [ks], lambda e: e.tensor_scalar(out=ks[:, 2:3], in0=ks[:, 1:2], scalar1=LN_EPS, scalar2=None, op0=ALU.add))
                        k.op("act", [ks], [ks], lambda e: e.activation(out=ks[:, 3:4], in_=ks[:, 2:3], func=AF.Sqrt))
                        k.op("dve", [ks], [ks], lambda e: e.reciprocal(out=ks[:, 2:3], in_=ks[:, 3:4]))
                        k.op("dve", [vx, ks], [kn], lambda e: e.tensor_scalar(out=kn[:], in0=vx[:, 128:192], scalar1=ks[:, 0:1], scalar2=ks[:, 2:3], op0=ALU.subtract, op1=ALU.mult))
                        k.op("pool", [kn, ig], [kn], lambda e: e.tensor_tensor(out=kn[:], in0=kn[:], in1=ig[:], op=ALU.mult))
                        k.op("pool", [kn, ib], [kn], lambda e: e.tensor_tensor(out=kn[:], in0=kn[:], in1=ib[:], op=ALU.add))
                        k.op("pool", [kn, ck], [tmp], lambda e: e.tensor_tensor(out=tmp[:, 0:16], in0=kn[:, 0:16], in1=ck[:], op=ALU.mult))
                        k.op("pool", [kn, sk], [tmp], lambda e: e.tensor_tensor(out=tmp[:, 16:32], in0=kn[:, 16:32], in1=sk[:], op=ALU.mult))
                        k.op("pool", [kn, ck], [tmp], lambda e: e.tensor_tensor(out=tmp[:, 32:48], in0=kn[:, 16:32], in1=ck[:], op=ALU.mult))
                        k.op("pool", [kn, sk], [tmp], lambda e: e.tensor_tensor(out=tmp[:, 48:64], in0=kn[:, 0:16], in1=sk[:], op=ALU.mult))
                        k.op("pool", [tmp], [kd], lambda e: e.tensor_tensor(out=kd[:, 0:16], in0=tmp[:, 0:16], in1=tmp[:, 16:32], op=ALU.subtract))
                        k.op("pool", [tmp], [kd], lambda e: e.tensor_tensor(out=kd[:, 16:32], in0=tmp[:, 32:48], in1=tmp[:, 48:64], op=ALU.add))
                        k.op("pool", [kn], [kd], lambda e: e.tensor_copy(kd[:, 32:64], kn[:, 32:64]))
                        k.op("pool", [kd], [kd], lambda e: e.tensor_copy(kd[:, 64:128], kd[:, 0:64]))
                        k.op("pe", [kd, ident], [psT], lambda e: e.transpose(psT[:], kd[:], ident[:]))
                        k.op("act", [psT], [kiT], lambda e: e.activation(out=kiT[:], in_=psT[:], func=AF.Copy))
                        k.dma("pool", KIT[:, ti * 128:(ti + 1) * 128], kiT[:], [kiT], [], kiT)
                        k.op("pool", [vx], [wi_], lambda e: e.tensor_scalar(out=wi_[:], in0=vx[:, 192:200], scalar1=IDX_W_SCALE * IDX_SCALE, scalar2=None, op0=ALU.mult))
                        k.dma("pool", WI[ti * 128:(ti + 1) * 128, :], wi_[:], [wi_], [], wi_)
                    vcnt += 1

    def phase_attn(L, lnidx):
        isb = (L == "b")
        with Phase(k) as ph:
            epi = Epi(ph, lnidx, nb=(1 if isb else 2))
            wo_sb = ph.sb([64, 16, D], BF16, "wo")
            k.dma("pool", wo_sb[:], W["wo_" + L].rearrange("(h d) n -> d h n", d=64), [], [wo_sb], wo_sb)
            mcur = ph.sb([128, 128], BF16, "mcur")
            k.dma("pool", mcur[:], mcur_d[:, :], [], [mcur], mcur)
            mprev = ph.sb([128, 128], BF16, "mprev")
            k.dma("pool", mprev[:], mprev_d[:, :], [], [mprev], mprev)
            mmeta = ph.sb([16, 16], BF16, "mmeta")
            k.dma("pool", mmeta[:], mmeta_d[:, :], [], [mmeta], mmeta)
            ones = ph.sb([128, 64], F32, "ones")
            k.op("dve", [], [ones], lambda e: e.memset(ones[:], 1.0))
            esrow = None
            if not isb:
                sraw = ph.sb([128, 16], F32, "sraw")
                k.dma("sp", sraw[64:65, :], sinks[0:1, :], [], [sraw], sraw)
                k.op("act", [sraw], [sraw], lambda e: e.activation(out=sraw[64:65, :], in_=sraw[64:65, :], func=AF.Exp))
                esrow = ph.sb([128, 16, 128], F32, "esrow")
                k.op("dve", [sraw], [esrow], lambda e: e.tensor_copy(esrow[64:65, :, :], apx(sraw[64:65, :], [[1, 16], [0, 128]])))
            else:
                negu = ph.sb([128, 128], F32, "negu")
                k.dma("sp", negu[:], negu_d[:, :], [], [negu], negu)
                idb = ph.sb([128, 128], BF16, "idb2")
                k.dma("pool", idb[:], ident_d[:, :], [], [idb], idb)
            attnT_m = ph.sb([64, 16, 48], BF16, "attnTm")
            k.op("dve", [], [attnT_m], lambda e: e.memset(attnT_m[:], 0.0))
            qTm = ph.sb([128, 8, 128], BF16, "qTm")
            k.dma("sp", qTm[:], QT[MT].rearrange("p (j t) -> p j t", j=8), [], [qTm], qTm)
            qTs = [ph.sb([128, 1024], BF16, "qT") for _ in range(2)]
            hs = [ph.sb([128, D], F32, "h") for _ in range(2)]
            zs = [ph.sb([128, D], F32, "z") for _ in range(2)]
            attnTs = [ph.sb([64, 16, 128], BF16, "attnT") for _ in range(2)]
            PTs = [ph.sb([128, 512], BF16, "PT") for _ in range(3)]
            rrs = [ph.sb([128, 512], F32, "rr") for _ in range(2)]
            oSs = [ph.sb([64, 512], F32, "oS") for _ in range(2)]
            bcSs = [ph.sb([64, 512], F32, "bcS") for _ in range(2)]
            kTm = ph.sb([128, 16], BF16, "kTm")
            Vm = ph.sb([16, 130], BF16, "Vm")
            psS = [ph.ps([128, 512], F32, "psS") for _ in range(2)]
            psO = [ph.ps([128, 512], F32, "psO") for _ in range(1 if isb else 2)]
            psBc = ph.ps([128, 512], F32, "psBc")
            psM = ph.ps([128, 512], F32, "psM")
            if isb:
                kT_all = ph.sb([128, S], BF16, "kTall")
                kiT_all = ph.sb([128, S], BF16, "kiTall")
                V_all = ph.sb([128, NB, 130], BF16, "Vall")
                scores = [ph.sb([128, S], F32, "score") for _ in range(2)]
                maskbs = [ph.sb([128, S], BF16, "maskb") for _ in range(2)]
                maskTs = [ph.sb([128, S], BF16, "maskT") for _ in range(2)]
                rls = [ph.sb([128, 512], F32, "rl") for _ in range(2)]
                qiTs = [ph.sb([128, 512], BF16, "qiT") for _ in range(2)]
                wits = [ph.sb([128, 8], F32, "wit") for _ in range(2)]
                nbias = ph.sb([128, 1], F32, "nbias")
                k.op("dve", [], [nbias], lambda e: e.memset(nbias[:], NEGM))
                score_gs = [[TB(None) for _ in range((S + 511) // 512)] for _ in range(2)]
                bstate = [[ph.sb([128, 1], F32, "bs") for _ in range(8)] + [ph.sb([128, N_BISECT + 1], F32, "Dk")] for _ in range(2)]
                pow2 = ph.sb([128, N_BISECT + 1], F32, "pow2")
                for kk in range(N_BISECT + 1):
                    k.op("dve", [], [pow2], lambda e, kk=kk: e.memset(pow2[:, kk:kk + 1], 2.0 ** -(kk + 1)))
                psI = ph.ps([128, 512], F32, "psI")
                psIs = [psI, psM]
                psTm = ph.ps([128, 512], BF16, "psTm")
            else:
                kT2s = [ph.sb([128, 256], BF16, "kT2") for _ in range(2)]
                V2s = [ph.sb([128, 2, 130], BF16, "V2") for _ in range(2)]
            cS = 0
            cP = 0
            cO = 0
            cM = 0

            def attend(qrhs, ncol, keytiles, g, sink_ap, out_ap):
                nonlocal cS, cP, cO, cM
                po = psO[cO % len(psO)]
                rr = rrs[cO % 2]
                oS = oSs[cO % 2]
                cO += 1
                LA = 2
                nkt = len(keytiles)
                pts = [None] * nkt
                for idx in range(nkt + LA):
                    if idx < nkt:
                        kap, vap, nk, mask, rds, mrds = keytiles[idx][:6]
                        mbias = keytiles[idx][6] if len(keytiles[idx]) > 6 else None
                        ps = psS[cS % 2]
                        cS += 1
                        PT = PTs[cP % 3]
                        cP += 1
                        pts[idx] = PT
                        k.op("pe", rds + qrhs[1], [ps], lambda e: e.matmul(ps[0:nk, 0:ncol], kap, qrhs[0], start=True, stop=(mbias is None)))
                        if mbias is not None:
                            k.op("pe", mrds + [idb], [ps], lambda e: e.matmul(ps[0:nk, 0:ncol], idb[0:nk, 0:nk], mbias, start=False, stop=True))
                        k.op("act", [ps], [PT], lambda e: e.activation(out=PT[0:nk, 0:ncol], in_=ps[0:nk, 0:ncol], func=AF.Exp, scale=ATTN_SCALE))
                        if mask is not None:
                            eng = "pool" if (isb or cM % 2 == 0) else "dve"
                            cM += 1
                            k.op(eng, [PT] + mrds, [PT], lambda e: e.tensor_tensor(out=PT[0:nk, 0:ncol], in0=PT[0:nk, 0:ncol], in1=mask, op=ALU.mult))
                    j = idx - LA
                    if j >= 0:
                        kap, vap, nk, mask, rds, mrds = keytiles[j][:6]
                        PT = pts[j]
                        k.op("pe", rds + [PT], [po], lambda e: e.matmul(po[0:65, 0:ncol], vap, PT[0:nk, 0:ncol], start=(j == 0), stop=(j == nkt - 1)))
                if sink_ap is not None:
                    k.op("dve", [po, esrow], [rr], lambda e: e.tensor_tensor(out=rr[64:65, 0:ncol], in0=po[64:65, 0:ncol], in1=sink_ap, op=ALU.add))
                    k.op("dve", [rr], [rr], lambda e: e.reciprocal(out=rr[64:65, 0:ncol], in_=rr[64:65, 0:ncol]))
                elif isb:
                    bcS = bcSs[cO % 2]
                    k.op("act", [po], [rr], lambda e: e.activation(out=rr[64:65, 0:ncol], in_=po[64:65, 0:ncol], func=AF.Copy))
                    k.op("act", [rr], [rr], lambda e: e.activation(out=rr[64:65, 0:ncol], in_=rr[64:65, 0:ncol], func=AF.Ln))
                    k.op("act", [rr], [rr], lambda e: e.activation(out=rr[64:65, 0:ncol], in_=rr[64:65, 0:ncol], func=AF.Exp, scale=-1.0))
                    k.op("pe", [ones, rr], [psBc], lambda e: e.matmul(psBc[0:64, 0:ncol], ones[64:65, 0:64], rr[64:65, 0:ncol], start=True, stop=True))
                    k.op("act", [psBc], [bcS], lambda e: e.activation(out=bcS[0:64, 0:ncol], in_=psBc[0:64, 0:ncol], func=AF.Copy))
                    k.op("act", [po], [oS], lambda e: e.activation(out=oS[0:64, 0:ncol], in_=po[0:64, 0:ncol], func=AF.Copy))
                    k.op("pool", [oS, bcS], out_ap[1], lambda e: e.tensor_tensor(out=out_ap[0], in0=oS[0:64, 0:ncol], in1=bcS[0:64, 0:ncol], op=ALU.mult))
                    return
                else:
                    k.op("dve", [po], [rr], lambda e: e.reciprocal(out=rr[64:65, 0:ncol], in_=po[64:65, 0:ncol]))
                k.op("pe", [ones, rr], [psBc], lambda e: e.matmul(psBc[0:64, 0:ncol], ones[64:65, 0:64], rr[64:65, 0:ncol], start=True, stop=True))
                k.op("act", [po], [oS], lambda e: e.activation(out=oS[0:64, 0:ncol], in_=po[0:64, 0:ncol], func=AF.Copy))
                k.op("dve", [oS, psBc], out_ap[1], lambda e: e.tensor_tensor(out=out_ap[0], in0=oS[0:64, 0:ncol], in1=psBc[0:64, 0:ncol], op=ALU.mult))

            def meta_queries(b):
                for g in range(2):
                    gs = slice(g * 64, (g + 1) * 64)
                    kts = [(kTm[gs, 0:16], Vm[0:16, g * 65:(g + 1) * 65], 16, apx(mmeta[:, :], [[0, 8], [1, 16]]), [kTm, Vm], [mmeta])]
                    sink_ap = None if isb else esrow[64:65, g * 8:(g + 1) * 8, 0:16]
                    attend((qTm[gs, :, 32 * b:32 * b + 16], [qTm]), 128, kts, g, sink_ap,
                           (attnT_m[0:64, g * 8:(g + 1) * 8, 32 * b:32 * b + 16], [attnT_m]))

            def out_proj_epi(attnT, h, z, ti):
                for dh in range(2):
                    for hd in range(16):
                        k.op("pe", [attnT, wo_sb], [psM], lambda e, hd=hd: e.matmul(psM[:, :], attnT[0:64, hd, :], wo_sb[0:64, hd, dh * 512:(dh + 1) * 512], start=(hd == 0), stop=(hd == 15)))
                    k.op("dve", [h, psM], [z], lambda e: e.scalar_tensor_tensor(out=z[:, dh * 512:(dh + 1) * 512], in0=h[:, dh * 512:(dh + 1) * 512], scalar=ALPHA,
                                                                                in1=psM[:, :], op0=ALU.mult, op1=ALU.add))
                epi.run(z, ti)

            if isb:
                blocks = [(b, n) for b in range(2) for n in range(NB)]
                ri_box = [0]

                def A1(i):
                    b, n = blocks[i]
                    p = i % 2
                    ti = b * NB + n
                    Wk = (n + 1) * 128
                    score, score_g = scores[p], score_gs[p]
                    qiT, wit = qiTs[p], wits[p]
                    bst = bstate[p]
                    if n == 0:
                        k.dma("sp", kiT_all[:], KIT[:, b * S:(b + 1) * S], [], [kiT_all], kiT_all)
                    k.dma("sp", qiT[:], QIT[ti], [], [qiT], qiT)
                    k.dma("sp", wit[:], WI[ti * 128:(ti + 1) * 128, :], [], [wit], wit)
                    ngr = (Wk + 511) // 512
                    sgs = score_g[0:ngr]
                    for hh in range(8):
                        j, half = hh % 4, hh // 4
                        for gi_ in range(ngr):
                            kg0 = gi_ * 512
                            w = min(512, Wk - kg0)
                            sg_ = score_g[gi_]
                            rl = rls[ri_box[0] % 2]
                            pI = psIs[ri_box[0] % 2]
                            ri_box[0] += 1
                            k.op("pe", [qiT, kiT_all], [pI], lambda e: e.matmul(pI[:, 0:w], qiT[half * 64:(half + 1) * 64, j * 128:(j + 1) * 128],
                                                                                kiT_all[half * 64:(half + 1) * 64, kg0:kg0 + w], start=True, stop=True))
                            k.op("act", [pI], [rl], lambda e: e.activation(out=rl[:, 0:w], in_=pI[:, 0:w], func=AF.Relu))
                            if hh == 0:
                                k.op("dve", [rl, wit], [sg_], lambda e: e.tensor_scalar(out=score[:, kg0:kg0 + w], in0=rl[:, 0:w], scalar1=wit[:, 0:1], scalar2=None, op0=ALU.mult))
                            else:
                                k.op("dve", [rl, wit, sg_], [sg_], lambda e: e.scalar_tensor_tensor(out=score[:, kg0:kg0 + w], in0=rl[:, 0:w], scalar=wit[:, hh:hh + 1],
                                                                                                    in1=score[:, kg0:kg0 + w], op0=ALU.mult, op1=ALU.add))
                    b_mx, b_mn, b_d, b_a0, b_a1, b_mid, b_cnt, b_tmp, Dk = bst
                    k.op("dve", sgs, [b_mx], lambda e: e.tensor_reduce(out=b_mx[:, 0:1], in_=score[:, 0:Wk], axis=AX.X, op=ALU.max))
                    k.op("dve", sgs, [b_mn], lambda e: e.tensor_reduce(out=b_mn[:, 0:1], in_=score[:, 0:Wk], axis=AX.X, op=ALU.min))
                    dg = score_g[n // 4]
                    k.op("dve", [dg, negu], [dg], lambda e: e.tensor_tensor(out=score[:, n * 128:(n + 1) * 128], in0=score[:, n * 128:(n + 1) * 128], in1=negu[:], op=ALU.add))
                    k.op("dve", [b_mx, b_mn], [b_d], lambda e: e.tensor_tensor(out=b_d[:, 0:1], in0=b_mx[:, 0:1], in1=b_mn[:, 0:1], op=ALU.subtract))
                    k.op("dve", [b_d], [b_d], lambda e: e.tensor_scalar(out=b_d[:, 0:1], in0=b_d[:, 0:1], scalar1=2.0, scalar2=None, op0=ALU.add))
                    k.op("dve", [b_mn], [b_a0], lambda e: e.tensor_scalar(out=b_a0[:, 0:1], in0=b_mn[:, 0:1], scalar1=-1.0, scalar2=None, op0=ALU.add))
                    k.op("dve", [pow2, b_d], [Dk], lambda e: e.tensor_scalar(out=Dk[:], in0=pow2[:], scalar1=b_d[:, 0:1], scalar2=None, op0=ALU.mult))
                    k.op("dve", [b_a0, Dk], [b_mid], lambda e: e.tensor_tensor(out=b_mid[:, 0:1], in0=b_a0[:, 0:1], in1=Dk[:, 0:1], op=ALU.add))

                def A2(i):
                    b, n = blocks[i]
                    p = i % 2
                    Wk = (n + 1) * 128
                    score, score_g, junk = scores[p], score_gs[p], maskbs[p]
                    sgs = score_g[0:(Wk + 511) // 512]
                    b_mx, b_mn, b_d, b_a0, b_a1, b_mid, b_cnt, b_tmp, Dk = bstate[p]
                    b_as = [b_a0, b_a1]
                    for it in range(N_BISECT):
                        k.op("dve", sgs + [b_mid], [junk, b_cnt], lambda e: e.tensor_scalar(out=junk[:, 0:Wk], in0=score[:, 0:Wk], scalar1=b_mid[:, 0:1], scalar2=0.0,
                                                                                         op0=ALU.is_ge, op1=ALU.add, accum_out=b_cnt[:, 0:1]))
                        k.op("dve", [b_cnt, Dk], [b_tmp], lambda e: e.scalar_tensor_tensor(out=b_tmp[:, 0:1], in0=b_cnt[:, 0:1], scalar=float(TOPK) - 0.5,
                                                                                          in1=Dk[:, it:it + 1], op0=ALU.is_ge, op1=ALU.mult))
                        b_a, b_an = b_as[it % 2], b_as[(it + 1) % 2]
                        if it < N_BISECT - 1:
                            k.op("dve", [b_tmp, b_a, Dk], [b_mid], lambda e: e.tensor_scalar(out=b_mid[:, 0:1], in0=b_tmp[:, 0:1], scalar1=b_a[:, 0:1],
                                                                                           scalar2=Dk[:, it + 1:it + 2], op0=ALU.add, op1=ALU.add))
                        k.op("dve", [b_tmp, b_a], [b_an], lambda e: e.tensor_tensor(out=b_an[:, 0:1], in0=b_a[:, 0:1], in1=b_tmp[:, 0:1], op=ALU.add))

                def A3(i):
                    b, n = blocks[i]
                    p = i % 2
                    Wk = (n + 1) * 128
                    score, score_g, maskb, maskT = scores[p], score_gs[p], maskbs[p], maskTs[p]
                    sgs = score_g[0:(Wk + 511) // 512]
                    b_a = bstate[p][3 + (N_BISECT % 2)]
                    k.op("pool", sgs + [b_a], [maskb], lambda e: e.tensor_scalar(out=maskb[:, 0:Wk], in0=score[:, 0:Wk], scalar1=b_a[:, 0:1], scalar2=None, op0=ALU.is_ge))
                    for m0 in range(0, n + 1, 4):
                        mc = min(4, n + 1 - m0)
                        for mm in range(mc):
                            k.op("pe", [maskb, idb], [psTm], lambda e, mm=mm: e.transpose(psTm[:, mm * 128:(mm + 1) * 128], maskb[:, (m0 + mm) * 128:(m0 + mm + 1) * 128], idb[:]))
                        k.op("act", [psTm, nbias], [maskT], lambda e: e.activation(out=maskT[:, m0 * 128:(m0 + mc) * 128], in_=psTm[:, 0:mc * 128], func=AF.Identity,
                                                                                       bias=nbias[:, 0:1], scale=-NEGM))

                def Bst(i):
                    b, n = blocks[i]
                    p = i % 2
                    ti = b * NB + n
                    maskT = maskTs[p]
                    qT, h, z, attnT = qTs[p], hs[p], zs[p], attnTs[p]
                    if n == 0:
                        mrow = 2 * S + 32 * b
                        k.dma("sp", kTm[:], KT[:, mrow:mrow + 16], [], [kTm], kTm)
                        k.dma("sp", Vm[:], VA[mrow:mrow + 16, :], [], [Vm], Vm)
                        k.dma("sp", kT_all[:], KT[:, b * S:(b + 1) * S], [], [kT_all], kT_all)
                        k.dma("sp", V_all[:], VA[b * S:(b + 1) * S, :].rearrange("(n p) c -> p n c", p=128), [], [V_all], V_all)
                    k.dma("sp", qT[:], QT[ti], [], [qT], qT)
                    k.dma("sp", h[:], H[ti * 128:(ti + 1) * 128, :], [], [h], h)
                    for g in range(2):
                        gs = slice(g * 64, (g + 1) * 64)
                        for half in range(2):
                            kts = [(kTm[gs, 0:16], Vm[0:16, g * 65:(g + 1) * 65], 16, None, [kTm, Vm], [])]
                            for m in range(n + 1):
                                kts.append((kT_all[gs, m * 128:(m + 1) * 128], V_all[:, m, g * 65:(g + 1) * 65], 128,
                                            None, [kT_all, V_all], [maskT], apx(maskT[:, m * 128:(m + 1) * 128], [[0, 4], [1, 128]])))
                            hd0 = g * 8 + half * 4
                            attend((qT[gs, half * 512:(half + 1) * 512], [qT]), 512, kts, g, None,
                                   (attnT[0:64, hd0:hd0 + 4, :].rearrange("p h t -> p (h t)"), [attnT]))
                    out_proj_epi(attnT, h, z, ti)
                    if n == NB - 1:
                        meta_queries(b)

                A1(0)
                A2(0)
                A3(0)
                for i in range(len(blocks)):
                    if i + 1 < len(blocks):
                        A1(i + 1)
                        A2(i + 1)
                    Bst(i)
                    if i + 1 < len(blocks):
                        A3(i + 1)
                blk = len(blocks)
            else:
                blk = 0
                for b in range(2):
                    mrow = 2 * S + 32 * b
                    k.dma("sp", kTm[:], KT[:, mrow:mrow + 16], [], [kTm], kTm)
                    k.dma("sp", Vm[:], VA[mrow:mrow + 16, :], [], [Vm], Vm)
                    if isb:
                        k.dma("sp", kT_all[:], KT[:, b * S:(b + 1) * S], [], [kT_all], kT_all)
                        k.dma("sp", kiT_all[:], KIT[:, b * S:(b + 1) * S], [], [kiT_all], kiT_all)
                        k.dma("sp", V_all[:], VA[b * S:(b + 1) * S, :].rearrange("(n p) c -> p n c", p=128), [], [V_all], V_all)
                    for n in range(NB):
                        ti = b * NB + n
                        qT, h, z, attnT = qTs[blk % 2], hs[blk % 2], zs[blk % 2], attnTs[blk % 2]
                        k.dma("sp", qT[:], QT[ti], [], [qT], qT)
                        k.dma("sp", h[:], H[ti * 128:(ti + 1) * 128, :], [], [h], h)
                        if not isb:
                            kT2, V2 = kT2s[blk % 2], V2s[blk % 2]
                            if n > 0:
                                k.dma("sp", kT2[:], KT[:, (ti - 1) * 128:(ti + 1) * 128], [], [kT2], kT2)
                                k.dma("sp", V2[:], VA[(ti - 1) * 128:(ti + 1) * 128, :].rearrange("(n p) c -> p n c", p=128), [], [V2], V2)
                            else:
                                k.dma("sp", kT2[:, 128:256], KT[:, ti * 128:(ti + 1) * 128], [], [kT2], kT2)
                                k.dma("sp", V2[:, 1, :], VA[ti * 128:(ti + 1) * 128, :], [], [V2], V2)
                        else:
                            Wk = (n + 1) * 128
                            qiT, wit = qiTs[blk % 2], wits[blk % 2]
                            k.dma("sp", qiT[:], QIT[ti], [], [qiT], qiT)
                            k.dma("sp", wit[:], WI[ti * 128:(ti + 1) * 128, :], [], [wit], wit)
                            ri = 0
                            ngr = (Wk + 511) // 512
                            sgs = score_g[0:ngr]
                            for hh in range(8):
                                j, half = hh % 4, hh // 4
                                for gi_ in range(ngr):
                                    kg0 = gi_ * 512
                                    w = min(512, Wk - kg0)
                                    sg_ = score_g[gi_]
                                    rl = rls[ri % 2]
                                    pI = psIs[ri % 2]
                                    ri += 1
                                    k.op("pe", [qiT, kiT_all], [pI], lambda e: e.matmul(pI[:, 0:w], qiT[half * 64:(half + 1) * 64, j * 128:(j + 1) * 128],
                                                                                        kiT_all[half * 64:(half + 1) * 64, kg0:kg0 + w], start=True, stop=True))
                                    k.op("act", [pI], [rl], lambda e: e.activation(out=rl[:, 0:w], in_=pI[:, 0:w], func=AF.Relu))
                                    if hh == 0:
                                        k.op("dve", [rl, wit], [sg_], lambda e: e.tensor_scalar(out=score[:, kg0:kg0 + w], in0=rl[:, 0:w], scalar1=wit[:, 0:1], scalar2=None, op0=ALU.mult))
                                    else:
                                        k.op("dve", [rl, wit, sg_], [sg_], lambda e: e.scalar_tensor_tensor(out=score[:, kg0:kg0 + w], in0=rl[:, 0:w], scalar=wit[:, hh:hh + 1],
                                                                                                            in1=score[:, kg0:kg0 + w], op0=ALU.mult, op1=ALU.add))
                            k.op("dve", sgs, [b_mx], lambda e: e.tensor_reduce(out=b_mx[:, 0:1], in_=score[:, 0:Wk], axis=AX.X, op=ALU.max))
                            k.op("dve", sgs, [b_mn], lambda e: e.tensor_reduce(out=b_mn[:, 0:1], in_=score[:, 0:Wk], axis=AX.X, op=ALU.min))
                            dg = score_g[n // 4]
                            k.op("dve", [dg, negu], [dg], lambda e: e.tensor_tensor(out=score[:, n * 128:(n + 1) * 128], in0=score[:, n * 128:(n + 1) * 128], in1=negu[:], op=ALU.add))
                            k.op("dve", [b_mx, b_mn], [b_d], lambda e: e.tensor_tensor(out=b_d[:, 0:1], in0=b_mx[:, 0:1], in1=b_mn[:, 0:1], op=ALU.subtract))
                            k.op("dve", [b_d], [b_d], lambda e: e.tensor_scalar(out=b_d[:, 0:1], in0=b_d[:, 0:1], scalar1=2.0, scalar2=None, op0=ALU.add))
                            b_a = b_as[0]
                            k.op("dve", [b_mn], [b_a], lambda e: e.tensor_scalar(out=b_a[:, 0:1], in0=b_mn[:, 0:1], scalar1=-1.0, scalar2=None, op0=ALU.add))
                            k.op("dve", [pow2, b_d], [Dk], lambda e: e.tensor_scalar(out=Dk[:], in0=pow2[:], scalar1=b_d[:, 0:1], scalar2=None, op0=ALU.mult))
                            k.op("dve", [b_a, Dk], [b_mid], lambda e: e.tensor_tensor(out=b_mid[:, 0:1], in0=b_a[:, 0:1], in1=Dk[:, 0:1], op=ALU.add))
                            for it in range(N_BISECT):
                                k.op("dve", sgs + [b_mid], [junk, b_cnt], lambda e: e.tensor_scalar(out=junk[:, 0:Wk], in0=score[:, 0:Wk], scalar1=b_mid[:, 0:1], scalar2=0.0,
                                                                                                 op0=ALU.is_ge, op1=ALU.add, accum_out=b_cnt[:, 0:1]))
                                k.op("dve", [b_cnt, Dk], [b_tmp], lambda e: e.scalar_tensor_tensor(out=b_tmp[:, 0:1], in0=b_cnt[:, 0:1], scalar=float(TOPK) - 0.5,
                                                                                                  in1=Dk[:, it:it + 1], op0=ALU.is_ge, op1=ALU.mult))
                                b_a, b_an = b_as[it % 2], b_as[(it + 1) % 2]
                                if it < N_BISECT - 1:
                                    k.op("dve", [b_tmp, b_a, Dk], [b_mid], lambda e: e.tensor_scalar(out=b_mid[:, 0:1], in0=b_tmp[:, 0:1], scalar1=b_a[:, 0:1],
                                                                                                   scalar2=Dk[:, it + 1:it + 2], op0=ALU.add, op1=ALU.add))
                                k.op("dve", [b_tmp, b_a], [b_an], lambda e: e.tensor_tensor(out=b_an[:, 0:1], in0=b_a[:, 0:1], in1=b_tmp[:, 0:1], op=ALU.add))
                            b_a = b_as[N_BISECT % 2]
                            k.op("pool", sgs + [b_a], [maskb], lambda e: e.tensor_scalar(out=maskb[:, 0:Wk], in0=score[:, 0:Wk], scalar1=b_a[:, 0:1], scalar2=None, op0=ALU.is_ge))
                            for m0 in range(0, n + 1, 4):
                                mc = min(4, n + 1 - m0)
                                for mm in range(mc):
                                    k.op("pe", [maskb, idb], [psTm], lambda e, mm=mm: e.transpose(psTm[:, mm * 128:(mm + 1) * 128], maskb[:, (m0 + mm) * 128:(m0 + mm + 1) * 128], idb[:]))
                                k.op("act", [psTm], [maskT], lambda e: e.activation(out=maskT[:, m0 * 128:(m0 + mc) * 128], in_=psTm[:, 0:mc * 128], func=AF.Copy))
                        for g in range(2):
                            gs = slice(g * 64, (g + 1) * 64)
                            for half in range(2):
                                kts = [(kTm[gs, 0:16], Vm[0:16, g * 65:(g + 1) * 65], 16, None, [kTm, Vm], [])]
                                if not isb:
                                    if n > 0:
                                        kts.append((kT2[gs, 0:128], V2[:, 0, g * 65:(g + 1) * 65], 128, apx(mprev[:, :], [[0, 4], [1, 128]]), [kT2, V2], [mprev]))
                                    kts.append((kT2[gs, 128:256], V2[:, 1, g * 65:(g + 1) * 65], 128, apx(mcur[:, :], [[0, 4], [1, 128]]), [kT2, V2], [mcur]))
                                else:
                                    for m in range(n + 1):
                                        kts.append((kT_all[gs, m * 128:(m + 1) * 128], V_all[:, m, g * 65:(g + 1) * 65], 128,
                                                    apx(maskT[:, m * 128:(m + 1) * 128], [[0, 4], [1, 128]]), [kT_all, V_all], [maskT]))
                                hd0 = g * 8 + half * 4
                                sink_ap = None if isb else esrow[64:65, hd0:hd0 + 4, :].rearrange("p h t -> p (h t)")
                                attend((qT[gs, half * 512:(half + 1) * 512], [qT]), 512, kts, g, sink_ap,
                                       (attnT[0:64, hd0:hd0 + 4, :].rearrange("p h t -> p (h t)"), [attnT]))
                        for dh in range(2):
                            for hd in range(16):
                                k.op("pe", [attnT, wo_sb], [psM], lambda e, hd=hd: e.matmul(psM[:, :], attnT[0:64, hd, :], wo_sb[0:64, hd, dh * 512:(dh + 1) * 512], start=(hd == 0), stop=(hd == 15)))
                            k.op("dve", [h, psM], [z], lambda e: e.scalar_tensor_tensor(out=z[:, dh * 512:(dh + 1) * 512], in0=h[:, dh * 512:(dh + 1) * 512], scalar=ALPHA,
                                                                                        in1=psM[:, :], op0=ALU.mult, op1=ALU.add))
                        epi.run(z, ti)
                        blk += 1
                    for g in range(2):
                        gs = slice(g * 64, (g + 1) * 64)
                        kts = [(kTm[gs, 0:16], Vm[0:16, g * 65:(g + 1) * 65], 16, apx(mmeta[:, :], [[0, 8], [1, 16]]), [kTm, Vm], [mmeta])]
                        sink_ap = None if isb else esrow[64:65, g * 8:(g + 1) * 8, 0:16]
                        attend((qTm[gs, :, 32 * b:32 * b + 16], [qTm]), 128, kts, g, sink_ap,
                               (attnT_m[0:64, g * 8:(g + 1) * 8, 32 * b:32 * b + 16], [attnT_m]))
            h, z = hs[blk % 2], zs[blk % 2]
            k.dma("sp", h[:], H[MT * 128:(MT + 1) * 128, :], [], [h], h)
            k.op("dve", [], [z], lambda e: e.memset(z[:], 0.0))
            for dh in range(2):
                for hd in range(16):
                    k.op("pe", [attnT_m, wo_sb], [psM], lambda e, hd=hd: e.matmul(psM[0:48, :], attnT_m[0:64, hd, :], wo_sb[0:64, hd, dh * 512:(dh + 1) * 512], start=(hd == 0), stop=(hd == 15)))
                k.op("dve", [h, psM], [z], lambda e: e.scalar_tensor_tensor(out=z[0:48, dh * 512:(dh + 1) * 512], in0=h[0:48, dh * 512:(dh + 1) * 512], scalar=ALPHA,
                                                                            in1=psM[0:48, :], op0=ALU.mult, op1=ALU.add))
            epi.run(z, MT)

    class TBView:
        def __init__(self, base, ap):
            object.__setattr__(self, "base", base)
            object.__setattr__(self, "ap", ap)

        def __getitem__(self, idx):
            return self.ap[idx]

        def __getattr__(self, n):
            return getattr(self.base, n)

        def __setattr__(self, n, v):
            setattr(self.base, n, v)

    for p in phases:
        if p == "wc":
            phase_wcast()
        elif p == "p0":
            phase_p0()
        elif p == "m0":
            phase_moe(0)
        elif p == "m1":
            phase_moe(1)
        elif p == "a1":
            phase_proj("a")
        elif p == "b1":
            phase_proj("b")
        elif p == "a2":
            phase_attn("a", 0)
        elif p == "b2":
            phase_attn("b", 2)
    k.barrier()
    k.es.close()
    return nc


def _rope_tables(pos, rot_dim, head_dim, reps):
    half = rot_dim // 2
    inv = ROPE_THETA ** (-np.arange(half, dtype=np.float32) / half)
    ang = pos.astype(np.float32)[None, :] * inv[:, None]
    cos, sin = np.cos(ang).astype(np.float32), np.sin(ang).astype(np.float32)
    C = np.ones((head_dim, pos.shape[0]), np.float32)
    Sg = np.zeros((head_dim, pos.shape[0]), np.float32)
    C[:half] = cos
    C[half:rot_dim] = cos
    Sg[:half] = -sin
    Sg[half:rot_dim] = sin
    return np.tile(C, (reps, 1)), np.tile(Sg, (reps, 1)), cos.T.copy(), sin.T.copy()


def _swap_cols(w, head_dim, rot_dim):
    half = rot_dim // 2
    n = w.shape[-1] // head_dim
    idx = np.arange(w.shape[-1]).reshape(n, head_dim).copy()
    a = idx[:, :half].copy()
    idx[:, :half] = idx[:, half:rot_dim]
    idx[:, half:rot_dim] = a
    return w[..., idx.reshape(-1)]


def _pair_cols(w, npair, head_dim):
    n = w.shape[-1] // head_dim
    idx = np.arange(w.shape[-1]).reshape(n, head_dim)
    order = np.stack([idx[:npair], idx[npair:]], 1).reshape(-1)
    return w[..., order]


def prep_shared(inp, S, NE):
    NB = S // 128
    NT = 2 * NB + 1
    R = NT * 128
    f = lambda a: np.ascontiguousarray(np.asarray(a, dtype=np.float32))
    pos = np.zeros(R, np.int64)
    for b in range(2):
        pos[b * S:(b + 1) * S] = N_META + np.arange(S)
        pos[2 * S + 32 * b:2 * S + 32 * b + 16] = np.arange(16)
    cq, sq, _, _ = _rope_tables(pos, 16, 64, 2)
    ci, si, ckt, skt = _rope_tables(pos, 32, 64, 2)
    sh = {"cq": cq, "sq": sq, "ci": ci, "si": si, "ckt": ckt, "skt": skt}
    ii = np.arange(128)
    sh["mcur"] = (ii[:, None] <= ii[None, :]).astype(np.float32)
    sh["mprev"] = (ii[:, None] > ii[None, :]).astype(np.float32)
    sh["mmeta"] = (np.arange(16)[:, None] <= np.arange(16)[None, :]).astype(np.float32)
    sh["negu"] = np.where(ii[None, :] > ii[:, None], NEGM, 0.0).astype(np.float32)
    sh["ident"] = np.eye(128, dtype=np.float32)
    sh["lng"] = f(np.stack([inp["ln_mix_g"][0], inp["ln_ffn_g"][0], inp["ln_mix_g"][1], inp["ln_ffn_g"][1]]))
    sh["lnb"] = f(np.stack([inp["ln_mix_b"][0], inp["ln_ffn_b"][0], inp["ln_mix_b"][1], inp["ln_ffn_b"][1]]))
    for L, wi, bi, wo in (("a", inp["w_in_a"][0], inp["b_in_a"][0], inp["w_out_a"][0]),
                          ("b", inp["w_in_b"][0], inp["b_in_b"][0], inp["w_out_b"][0])):
        wi = np.asarray(wi, np.float32)
        bi = np.asarray(bi, np.float32)
        wq, bq = wi[:, :1024], bi[:1024]
        wk, bk = wi[:, 1024:1152], bi[1024:1152]
        sh["wq_" + L] = f(_pair_cols(wq, 8, 64))
        sh["wqs_" + L] = f(_pair_cols(_swap_cols(wq, 64, 16), 8, 64))
        sh["bq_" + L] = f(_pair_cols(bq, 8, 64).reshape(8, 128).T)
        sh["bqs_" + L] = f(_pair_cols(_swap_cols(bq, 64, 16), 8, 64).reshape(8, 128).T)
        sh["wk_" + L] = f(wk)
        sh["wks_" + L] = f(_swap_cols(wk, 64, 16))
        sh["bk_" + L] = f(bk.reshape(128, 1))
        sh["bks_" + L] = f(_swap_cols(bk, 64, 16).reshape(128, 1))
        sh["wo_" + L] = f(wo)
    wa, ba = np.asarray(inp["w_in_a"][0], np.float32), np.asarray(inp["b_in_a"][0], np.float32)
    wb, bb = np.asarray(inp["w_in_b"][0], np.float32), np.asarray(inp["b_in_b"][0], np.float32)
    sh["wvx_a"] = f(wa[:, 1152:1280])
    sh["bvx_a"] = f(ba[1152:1280].reshape(1, 128))
    sh["wvx_b"] = f(np.concatenate([wb[:, 1152:1280], wb[:, 1792:1856], wb[:, 1856:1864]], 1))
    sh["bvx_b"] = f(np.concatenate([bb[1152:1280], bb[1792:1856], bb[1856:1864]]).reshape(1, 200))
    wqi, bqi = wb[:, 1280:1792], bb[1280:1792]
    sh["wqi"] = f(_pair_cols(wqi, 4, 64))
    sh["wqis"] = f(_pair_cols(_swap_cols(wqi, 64, 32), 4, 64))
    sh["bqi"] = f(_pair_cols(bqi, 4, 64).reshape(4, 128).T)
    sh["bqis"] = f(_pair_cols(_swap_cols(bqi, 64, 32), 4, 64).reshape(4, 128).T)
    sh["sinks"] = f(np.asarray(inp["sinks_a"][0]).reshape(1, 16))
    sh["idxg"] = f(np.asarray(inp["idx_k_norm_g"][0]).reshape(1, 64))
    sh["idxb"] = f(np.asarray(inp["idx_k_norm_b"][0]).reshape(1, 64))
    sh["wr"] = f(inp["w_router"])
    sh["br"] = f(inp["b_router"])
    for l in (0, 1):
        sh["wgu%d" % l] = f(inp["w_gate_up"][l]).reshape(NE * D, 2048)
        sh["wd%d" % l] = f(inp["w_down"][l]).reshape(NE * D, D)
    sh["bgu"] = f(np.asarray(inp["b_gate_up"], np.float32).reshape(2, NE, 16, 128).transpose(0, 3, 1, 2).reshape(2, 128, NE * 16))
    sh["bd"] = f(inp["b_down"])
    return sh


_NC_CACHE = {}
LAUNCHES = (("wc", "p0", "a1", "a2", "m0", "b1", "b2", "m1"),)


def _get_nc(S, NE, phases):
    key = (S, NE, tuple(phases))
    if key not in _NC_CACHE:
        _NC_CACHE[key] = build(S, NE, tuple(phases))
    return _NC_CACHE[key]


def run(inp, launches=LAUNCHES, trace=False):
    x = np.asarray(inp["x"], np.float32)
    Bt, S, _ = x.shape
    NE = inp["w_router"].shape[-1]
    assert Bt == 16
    NT = 2 * (S // 128) + 1
    R = NT * 128
    sh = prep_shared(inp, S, NE)
    meta = np.asarray(inp["meta_tokens"], np.float32)
    hins = []
    for c in range(8):
        h = np.zeros((R, D), np.float32)
        h[:2 * S] = x[2 * c:2 * c + 2].reshape(2 * S, D)
        h[2 * S:2 * S + 16] = meta
        h[2 * S + 32:2 * S + 48] = meta
        hins.append(h)
    res = None
    for phases in launches:
        nc = _get_nc(S, NE, phases)
        moe_l = [l for l in (0, 1) if ("m%d" % l) in phases]
        skip = set()
        for l in (0, 1):
            if l not in moe_l:
                skip.add("wgu%d" % l)
                skip.add("wd%d" % l)
        in_maps = []
        for c in range(8):
            m = {kk: v for kk, v in sh.items() if kk not in skip}
            m["hin"] = hins[c]
            in_maps.append(m)
        res = run_bass_kernel_spmd(nc, in_maps, core_ids=list(range(8)), **({"trace": True} if trace else {}))
        hins = [np.ascontiguousarray(np.asarray(r["out"], dtype=np.float32)) for r in res.results]
    return hins, res


def kernel(**inputs):
    outs, _ = run(inputs)
    S = inputs["x"].shape[1]
    y = np.concatenate([o[:2 * S].reshape(2, S, D) for o in outs], 0)
    return y.astype(np.float32, copy=False)
```
